# Optimizing a Trainium2 kernel written in Bass

```python
import jax, jax.numpy as jnp
from jax import lax
import numpy as np

D_MODEL = 1024
BATCH = 16
SEQ = 2048
DEPTH = 4

N_EVEN = (DEPTH + 1) // 2
N_ODD = DEPTH // 2
A_HEADS = 4
A_HEAD_DIM = 128
A_WIDTH = A_HEADS * A_HEAD_DIM
A_CHUNK = 128
B_GROUPS = 4
B_GROUP_DIM = 128
B_WIDTH = B_GROUPS * B_GROUP_DIM
B_WINDOWS = (2, 4, 8, 16)
EVEN_IN = 2 * A_WIDTH + B_WIDTH
EVEN_OUT = A_WIDTH + B_WIDTH
C_HEADS = 4
C_QK_DIM = 128
C_V_DIM = 256
C_QK_WIDTH = C_HEADS * C_QK_DIM
C_V_WIDTH = C_HEADS * C_V_DIM
C_CHUNK = 128
C_CONV = 4
ODD_IN = 2 * C_QK_WIDTH + 2 * C_V_WIDTH + 2 * C_HEADS
D_FF = ((8 * D_MODEL // 3 + 255) // 256) * 256
PLE_DIM = 256
EPS = 1e-6

kernel_name = "hybrid_gmlp_pool_mlstm_trunk"


def rmsnorm(x, g):
    xf = x.astype(jnp.float32)
    y = xf * lax.rsqrt(jnp.mean(xf * xf, axis=-1, keepdims=True) + EPS)
    return (y * g.astype(jnp.float32)).astype(x.dtype)


def gmlp_chunk_mixer(u, v, v_gain, w_s, b_s):
    bsz, s, _ = u.shape
    u = jax.nn.gelu(u)
    v = rmsnorm(jax.nn.gelu(v), v_gain)
    nc = s // A_CHUNK
    vc = v.reshape(bsz, nc, A_CHUNK, A_HEADS, A_HEAD_DIM)
    mask = jnp.tril(jnp.ones((A_CHUNK, A_CHUNK), dtype=bool))
    w = jnp.where(mask, w_s, 0).astype(v.dtype)
    sv = jnp.einsum('hts,bcshd->bcthd', w, vc) + b_s.T.astype(v.dtype)[:, :, None]
    return u * sv.reshape(bsz, s, A_WIDTH)


def multiscale_pool_mixer(xb, w_pool, pool_scale):
    bsz, s, _ = xb.shape
    xg = xb.astype(jnp.float32).reshape(bsz, s, B_GROUPS, B_GROUP_DIM)
    cs = jnp.cumsum(xg, axis=1)
    t = jnp.arange(s)
    outs = []
    for g, win in enumerate(B_WINDOWS):
        csg = cs[:, :, g]
        lag = jnp.pad(csg, ((0, 0), (win, 0), (0, 0)))[:, :s]
        cnt = jnp.minimum(t + 1, win).astype(jnp.float32)[None, :, None]
        outs.append((csg - lag) / cnt - xg[:, :, g])
    pooled = jnp.stack(outs, axis=2).astype(xb.dtype)
    y = jnp.einsum('bsgd,gde->bsge', pooled, w_pool).reshape(bsz, s, B_WIDTH)
    return y * pool_scale


def even_mixer(xn, w_in, a_v_gain, a_ws, a_bs, b_wpool, b_scale, w_out):
    z = xn @ w_in
    u, v, xb = jnp.split(z, [A_WIDTH, 2 * A_WIDTH], axis=-1)
    ya = gmlp_chunk_mixer(u, v, a_v_gain, a_ws, a_bs)
    yb = multiscale_pool_mixer(xb, b_wpool, b_scale)
    return jnp.concatenate([ya, yb], axis=-1) @ w_out


def causal_short_conv(x, w):
    s = x.shape[1]
    y = x * w[0]
    for k in range(1, C_CONV):
        y = y + jnp.pad(x, ((0, 0), (k, 0), (0, 0)))[:, :s] * w[k]
    return y


def mlstm_chunkwise(q, k, v, ig, lf):
    bsz, s = q.shape[:2]
    nc = s // C_CHUNK
    L = C_CHUNK

    def to_chunks(a):
        a = a.reshape((bsz, nc, L) + a.shape[2:])
        a = jnp.moveaxis(a, 1, 0)
        return jnp.swapaxes(a, 2, 3)

    qc, kc, vc, ic, fc = (to_chunks(a) for a in (q, k, v, ig, lf))
    mask = jnp.tril(jnp.ones((L, L), dtype=bool))

    def step(carry, inp):
        c_st, n_st, m_st = carry
        qb, kb, vb, ib, fb = inp
        b = jnp.cumsum(fb, axis=-1)
        dlog = jnp.where(mask, b[..., :, None] - b[..., None, :] + ib[..., None, :], -jnp.inf)
        a = b + m_st[..., None]
        mt = jnp.maximum(a, jnp.max(dlog, axis=-1))
        wi = jnp.exp(a - mt)
        sc = jnp.einsum('bhtd,bhsd->bhts', qb, kb) * jnp.exp(dlog - mt[..., None])
        num = wi[..., None] * jnp.einsum('bhtd,bhde->bhte', qb, c_st) + jnp.einsum('bhts,bhse->bhte', sc, vb)
        den = wi * jnp.einsum('bhtd,bhd->bht', qb, n_st) + jnp.sum(sc, axis=-1)
        h = num / jnp.maximum(jnp.abs(den), jnp.exp(-mt))[..., None]
        b_last = b[..., -1]
        g = b_last[..., None] - b + ib
        m_new = jnp.maximum(b_last + m_st, jnp.max(g, axis=-1))
        wc = jnp.exp(b_last + m_st - m_new)
        ws = jnp.exp(g - m_new[..., None])
        c_new = wc[..., None, None] * c_st + jnp.einsum('bhs,bhsd,bhse->bhde', ws, kb, vb)
        n_new = wc[..., None] * n_st + jnp.einsum('bhs,bhsd->bhd', ws, kb)
        return (c_new, n_new, m_new), h

    init = (jnp.zeros((bsz, C_HEADS, C_QK_DIM, C_V_DIM), jnp.float32),
            jnp.zeros((bsz, C_HEADS, C_QK_DIM), jnp.float32),
            jnp.zeros((bsz, C_HEADS), jnp.float32))
    _, hs = lax.scan(step, init, (qc, kc, vc, ic, fc))
    hs = jnp.moveaxis(jnp.swapaxes(hs, 2, 3), 0, 1)
    return hs.reshape(bsz, s, C_HEADS, C_V_DIM)


def odd_mixer(xn, w_in, conv_w, b_i, b_f, h_gain, w_out):
    bsz, s, _ = xn.shape
    z = xn @ w_in
    o1 = 2 * C_QK_WIDTH
    o2 = o1 + C_V_WIDTH
    o3 = o2 + C_V_WIDTH
    o4 = o3 + C_HEADS
    qk, v, o, gi, gf = jnp.split(z, [o1, o2, o3, o4], axis=-1)
    qk = jax.nn.silu(causal_short_conv(qk, conv_w))
    q, k = jnp.split(qk, 2, axis=-1)
    q = q.reshape(bsz, s, C_HEADS, C_QK_DIM).astype(jnp.float32) * (C_QK_DIM ** -0.5)
    k = k.reshape(bsz, s, C_HEADS, C_QK_DIM).astype(jnp.float32)
    v = v.reshape(bsz, s, C_HEADS, C_V_DIM).astype(jnp.float32)
    ig = (gi + b_i).astype(jnp.float32)
    lf = jax.nn.log_sigmoid((gf + b_f).astype(jnp.float32))
    h = mlstm_chunkwise(q, k, v, ig, lf)
    h = rmsnorm(h, h_gain.reshape(C_HEADS, C_V_DIM)).astype(xn.dtype).reshape(bsz, s, C_V_WIDTH)
    return (h * jax.nn.sigmoid(o)) @ w_out


def swiglu(x, w_gate_up, w_down):
    g, u = jnp.split(x @ w_gate_up, 2, axis=-1)
    return (jax.nn.silu(g) * u) @ w_down


def setup_inputs(seed: int = 0) -> dict:
    key = jax.random.key(seed)
    ks = iter(jax.random.split(key, 32))
    nrm = lambda shape, scale: jax.random.normal(next(ks), shape, jnp.float32) * scale
    gain = lambda shape: 1.0 + 0.1 * jax.random.normal(next(ks), shape, jnp.float32)
    b_f = (jnp.linspace(3.0, 6.0, C_HEADS, dtype=jnp.float32)[None, :]
           + 0.1 * jax.random.normal(next(ks), (N_ODD, C_HEADS), jnp.float32))
    return {
        "x": nrm((BATCH, SEQ, D_MODEL), 1.0),
        "p": nrm((DEPTH, BATCH, SEQ, PLE_DIM), 1.0),
        "mix_pre_gain": gain((DEPTH, D_MODEL)),
        "mix_post_gain": gain((DEPTH, D_MODEL)),
        "ffn_pre_gain": gain((DEPTH, D_MODEL)),
        "ffn_post_gain": gain((DEPTH, D_MODEL)),
        "ple_post_gain": gain((DEPTH, D_MODEL)),
        "even_w_in": nrm((N_EVEN, D_MODEL, EVEN_IN), D_MODEL ** -0.5),
        "even_a_v_gain": gain((N_EVEN, A_WIDTH)),
        "even_a_ws": nrm((N_EVEN, A_HEADS, A_CHUNK, A_CHUNK), 0.5 * A_CHUNK ** -0.5),
        "even_a_bs": gain((N_EVEN, A_HEADS, A_CHUNK)),
        "even_b_wpool": nrm((N_EVEN, B_GROUPS, B_GROUP_DIM, B_GROUP_DIM), B_GROUP_DIM ** -0.5),
        "even_b_scale": gain((N_EVEN, B_WIDTH)),
        "even_w_out": nrm((N_EVEN, EVEN_OUT, D_MODEL), EVEN_OUT ** -0.5),
        "odd_w_in": nrm((N_ODD, D_MODEL, ODD_IN), D_MODEL ** -0.5),
        "odd_conv_w": nrm((N_ODD, C_CONV, 2 * C_QK_WIDTH), C_CONV ** -0.5),
        "odd_b_i": nrm((N_ODD, C_HEADS), 0.1),
        "odd_b_f": b_f,
        "odd_h_gain": gain((N_ODD, C_V_WIDTH)),
        "odd_w_out": nrm((N_ODD, C_V_WIDTH, D_MODEL), C_V_WIDTH ** -0.5),
        "ffn_w_gate_up": nrm((DEPTH, D_MODEL, 2 * D_FF), D_MODEL ** -0.5),
        "ffn_w_down": nrm((DEPTH, D_FF, D_MODEL), D_FF ** -0.5),
        "ple_proj": nrm((DEPTH, PLE_DIM, D_MODEL), PLE_DIM ** -0.5),
        "ple_gate": nrm((DEPTH, D_MODEL, D_MODEL), D_MODEL ** -0.5),
    }


def reference(x, p, mix_pre_gain, mix_post_gain, ffn_pre_gain, ffn_post_gain, ple_post_gain,
              even_w_in, even_a_v_gain, even_a_ws, even_a_bs, even_b_wpool, even_b_scale, even_w_out,
              odd_w_in, odd_conv_w, odd_b_i, odd_b_f, odd_h_gain, odd_w_out,
              ffn_w_gate_up, ffn_w_down, ple_proj, ple_gate):
    for i in range(DEPTH):
        j = i // 2
        h = rmsnorm(x, mix_pre_gain[i])
        if i % 2 == 0:
            y = even_mixer(h, even_w_in[j], even_a_v_gain[j], even_a_ws[j], even_a_bs[j],
                           even_b_wpool[j], even_b_scale[j], even_w_out[j])
        else:
            y = odd_mixer(h, odd_w_in[j], odd_conv_w[j], odd_b_i[j], odd_b_f[j],
                          odd_h_gain[j], odd_w_out[j])
        x = x + rmsnorm(y, mix_post_gain[i])
        y = swiglu(rmsnorm(x, ffn_pre_gain[i]), ffn_w_gate_up[i], ffn_w_down[i])
        x = x + rmsnorm(y, ffn_post_gain[i])
        e = jax.nn.sigmoid(x @ ple_gate[i]) * (p[i] @ ple_proj[i])
        x = x + rmsnorm(e, ple_post_gain[i])
    return x
```

```python
import numpy as np
from contextlib import ExitStack
import concourse.bass as bass
import concourse.mybir as mybir
from concourse.bass_utils import run_bass_kernel_spmd

F32 = mybir.dt.float32
BF16 = mybir.dt.bfloat16
ALU = mybir.AluOpType
AF = mybir.ActivationFunctionType

ENGS = ("pe", "act", "dve", "pool", "sp")
NDMASEM = 8
EPS = 1e-6
NV = 236
NCONST = 1472
TP = 512
NSLOT = 6
WLOOK = 3
WSZ = 2048
NEGBIG = -30000.0
KFIRST = False


class Sched:
    def __init__(self, nc, sems):
        self.nc = nc
        self.ops = {e: [] for e in ENGS}
        self.sem, self.dsem = sems
        self.cnt = {e: 0 for e in ENGS}
        self.dcnt = {q: 0 for q in ("sp", "pool")}
        self.seen = {}
        self.last_w = {}
        self.readers = {}

    def _semh(self, k):
        return self.sem[k] if isinstance(k, str) else self.dsem[k[0]][k[1]]

    def _need(self, eng, ev, waits):
        if ev is None:
            return
        k, v = ev
        if k == "pe" and eng == "pe":
            return
        if self.seen.get((eng, k), 0) >= v:
            return
        if isinstance(k, str):
            assert v <= self.cnt[k], ("wait on un-incremented event", eng, k, v, self.cnt[k])
        self.seen[(eng, k)] = v
        waits.append((k, v))

    def _deps(self, eng, reads, writes):
        waits = []
        for r in reads:
            self._need(eng, self.last_w.get(r), waits)
        for w in writes:
            self._need(eng, self.last_w.get(w), waits)
            for ev in self.readers.get(w, ()):
                self._need(eng, ev, waits)
        return waits

    def _commit(self, ev, reads, writes):
        for r in reads:
            self.readers.setdefault(r, []).append(ev)
        for w in writes:
            self.last_w[w] = ev
            self.readers[w] = []

    def op(self, eng, fn, reads=(), writes=(), inc=True):
        waits = self._deps(eng, reads, writes)
        ev = (eng, self.cnt[eng] + 1)
        if inc:
            self.cnt[eng] += 1
        self.ops[eng].append((waits, fn, (eng, 1) if inc else None))
        self._commit(ev, reads, writes)
        return ev

    def dma(self, q, out, in_, reads=(), writes=()):
        i = self.dcnt[q]
        self.dcnt[q] += 1
        slot = i % NDMASEM
        k = (q, slot)
        waits = self._deps(q, reads, writes)
        prev = i // NDMASEM
        if prev > 0:
            self._need(q, (k, 16 * prev), waits)
        ev = (k, 16 * (prev + 1))
        self.ops[q].append((waits, lambda e: e.dma_start(out=out, in_=in_), (k, 16)))
        self._commit(ev, reads, writes)
        return ev

    def barrier(self, engs=("pe", "act", "dve", "pool")):
        for e in engs:
            waits = []
            for k in engs:
                if k != e and self.cnt[k] > 0:
                    self._need(e, (k, self.cnt[k]), waits)
            if waits:
                self.ops[e].append((waits, None, None))

    def finish(self, eng, evs):
        waits = []
        for ev in evs:
            self._need(eng, ev, waits)
        self.ops[eng].append((waits, None, None))

    def emit(self):
        nc = self.nc
        engmap = {"pe": "tensor", "act": "scalar", "dve": "vector", "pool": "gpsimd", "sp": "sync"}
        with nc.Block() as block:
            for e in ENGS:
                ops = self.ops[e]

                def body(engine, ops=ops):
                    for waits, fn, inc in ops:
                        for k, v in waits:
                            engine.wait_ge(self._semh(k), v)
                        if fn is None:
                            continue
                        ins = fn(engine)
                        if inc is not None:
                            ins.then_inc(self._semh(inc[0]), inc[1])

                getattr(block, engmap[e])(body)


def build_nc(NSEQ=2, T=2048, NL=4):
    nc = bass.Bass("TRN2", target_bir_lowering=False)
    D = 1024
    NPASS = T // TP
    NTT = T // 128

    def din(name, shape):
        return nc.dram_tensor(name, list(shape), F32, kind="ExternalInput").ap()

    x_d = din("x", [NSEQ, T, D])
    p_d = din("p", [4, NSEQ, T, 256])
    ewin = din("even_w_in", [2, 1024, 1536])
    ewout = din("even_w_out", [2, 1024, 1024])
    ewpool = din("even_b_wpool", [2, 4, 128, 128])
    wst_d = din("ws_t", [2, 4, 128, 128])
    owin = din("odd_w_in", [2, 1024, 3080])
    owout = din("odd_w_out", [2, 1024, 1024])
    wgu = din("ffn_w_gate_up", [4, 1024, 5632])
    wdn = din("ffn_w_down", [4, 2816, 1024])
    pproj = din("ple_proj", [4, 256, 1024])
    pgate = din("ple_gate", [4, 1024, 1024])
    vecs_d = din("vecs", [128, NV])
    bce_d = din("bc_even", [2, 128, 2560])
    bco_d = din("bc_odd", [2, 128, 1024])
    consts_d = din("consts", [128, NCONST])
    out_d = nc.dram_tensor("out", [NSEQ, T, D], F32, kind="ExternalOutput").ap()

    with ExitStack() as st:
        sems = ({e: st.enter_context(nc.semaphore("s_" + e)) for e in ENGS},
                {q: [st.enter_context(nc.semaphore("d_%s%d" % (q, i))) for i in range(NDMASEM)] for q in ("sp", "pool")})

        def sb(n, sh, dt):
            return st.enter_context(nc.sbuf_tensor("sb_" + n, list(sh), dt))

        xT = sb("xT", [128, 8, T], F32)
        cst = sb("cst", [128, NCONST], F32)
        vecs = sb("vecs", [128, NV], F32)
        nbf = sb("nbf", [128, 2], F32)
        lbc = sb("lbc", [128, 2560], F32)
        identb = sb("identb", [128, 128], BF16)
        onesb = sb("onesb", [128, 128], BF16)
        hT = sb("hT", [128, 8, TP], BF16)
        ybuf = sb("ybuf", [128, 8, TP], F32)
        sq = sb("sq", [128, 8, TP], BF16)
        rstd = sb("rstd", [128, TP], F32)
        tA = sb("tA", [128, TP], F32)
        tB = [sb("tB%d" % i, [128, TP], F32) for i in range(2)]
        wsl = [sb("wsl%d" % i, [128, WSZ], BF16) for i in range(NSLOT)]
        pin = sb("pin", [128, 4, 256], BF16)
        pT = sb("pT", [128, 2, TP], BF16)
        WT = sb("WT", [128, 4, 128], BF16)
        wstf = sb("wstf", [128, 4, 128], F32)
        halo_e = sb("halo_e", [128, 4, 16], F32)
        halo_o = sb("halo_o", [128, 8, 4], F32)
        Cst = sb("Cst", [128, 4, 257], F32)
        Cb = sb("Cb", [128, 4, 258], BF16)
        mprev = sb("mprev", [4, 8], F32)
        smal = sb("smal", [128, 64], F32)
        NA = 12288
        arena = sb("arena", [128, NA], F32)
        ps = [st.enter_context(nc.psum_tensor("ps%d" % i, [128, 512], F32)) for i in range(8)]
        psb = [p_[:].bitcast(BF16) for p_ in ps]
        def record(S, plan):
            bank = [0]

            def nb():
                b = bank[0]
                bank[0] = (b + 1) % 8
                return b

            class Ar:
                def __init__(self):
                    self.off = 0

                def f32(self, n):
                    a = arena[:, self.off:self.off + n]
                    self.off += n
                    assert self.off <= NA, self.off
                    return a

                def b16(self, n):
                    w = (n + 1) // 2
                    a = arena[:, self.off:self.off + w].bitcast(BF16)
                    self.off += w
                    assert self.off <= NA, self.off
                    return a

            ident = cst[:, 0:128]
            triu = cst[:, 128:256]
            maskneg4 = cst[:, 256:768]
            invcnt = cst[:, 1280:1344]

            def gcol(kind, l, c):
                return vecs[:, (kind * 4 + l) * 8 + c:(kind * 4 + l) * 8 + c + 1]

            def MM(b, lo, hi, pairs, reads, rows=128, preads=None):
                n = len(pairs)
                for i, (l, r) in enumerate(pairs):
                    rd = list(reads) if i == 0 else []
                    if preads is not None:
                        rd += list(preads[i])
                    S.op("pe", lambda e, l=l, r=r, i=i: e.matmul(ps[b][0:rows, lo:hi], lhsT=l, rhs=r,
                                                                  start=(i == 0), stop=(i == n - 1)),
                         reads=rd, writes=[("ps", b)], inc=(i == n - 1))

            wcount = [0]
            wissued = [0]
            wlist = []

            def wload(src_ap, view):
                i = wcount[0]
                wcount[0] += 1
                wlist.append((src_ap, view))
                if plan is None:
                    S.dma("pool", view(wsl[i % NSLOT]), src_ap, writes=[("w", i % NSLOT)])
                else:
                    while wissued[0] < min(i + WLOOK + 1, len(plan)):
                        n = wissued[0]
                        wissued[0] += 1
                        src_n, view_n = plan[n]
                        S.dma("pool", view_n(wsl[n % NSLOT]), src_n, writes=[("w", n % NSLOT)])
                return view(wsl[i % NSLOT]), ("w", i % NSLOT)

            def v3(k, n):
                return lambda s_: s_[:, 0:k * n].rearrange("p (k n) -> p k n", k=k)

            def norm_rstd(srcs, regs, Dn, presq=False):
                nch = len(srcs)
                if not presq:
                    for c in range(nch):
                        if c % 2 == 0:
                            S.op("act", lambda e, c=c: e.activation(out=sq[:, c, :], in_=srcs[c], func=AF.Square),
                                 reads=[regs[c]], writes=[("sq", c)])
                        else:
                            S.op("dve", lambda e, c=c: e.tensor_tensor(out=sq[:, c, :], in0=srcs[c], in1=srcs[c], op=ALU.mult),
                                 reads=[regs[c]], writes=[("sq", c)])
                b = nb()
                MM(b, 0, TP, [(onesb[:], sq[:, c, :]) for c in range(nch)], reads=["onesb"],
                   preads=[[("sq", c)] for c in range(nch)])
                S.op("act", lambda e: e.activation(out=tA[:], in_=ps[b][:], func=AF.Ln, scale=1.0 / Dn, bias=EPS),
                     reads=[("ps", b)], writes=["tA"])
                S.op("act", lambda e: e.activation(out=rstd[:], in_=tA[:], func=AF.Exp, scale=-0.5), reads=["tA"], writes=["rstd"])

            def xs(c, pi):
                return xT[:, c, pi * TP:(pi + 1) * TP]

            def prenorm(kind, l, pi, presq=False):
                norm_rstd([xs(c, pi) for c in range(8)], [("x", c, pi) for c in range(8)], D, presq=presq)
                for c in range(8):
                    S.op("dve", lambda e, c=c: e.scalar_tensor_tensor(out=hT[:, c, :], in0=xs(c, pi), scalar=gcol(kind, l, c),
                                                                      in1=rstd[:], op0=ALU.mult, op1=ALU.mult),
                         reads=[("x", c, pi), "rstd", "vecs"], writes=[("hT", c)])

            def ysq(mi, b):
                S.op("dve", lambda e, mi=mi, b=b: e.tensor_tensor(out=sq[:, mi, :], in0=ps[b][:], in1=ybuf[:, mi, :], op=ALU.mult),
                     reads=[("ps", b), ("ybuf", mi)], writes=[("sq", mi)])

            def postnorm_add(kind, l, pi, follow=None):
                norm_rstd([ybuf[:, m, :] for m in range(8)], [("ybuf", m) for m in range(8)], D, presq=True)
                for m in range(8):
                    t = tB[m % 2]
                    S.op("dve", lambda e, m=m, t=t: e.scalar_tensor_tensor(out=t[:], in0=ybuf[:, m, :], scalar=gcol(kind, l, m),
                                                                           in1=rstd[:], op0=ALU.mult, op1=ALU.mult),
                         reads=[("ybuf", m), "rstd", "vecs"], writes=[("tB", m % 2)])
                    S.op("dve", lambda e, m=m, t=t: e.tensor_tensor(out=xs(m, pi), in0=xs(m, pi), in1=t[:], op=ALU.add),
                         reads=[("tB", m % 2), ("x", m, pi)], writes=[("x", m, pi)])
                    if follow == "sq":
                        S.op("act", lambda e, m=m: e.activation(out=sq[:, m, :], in_=xs(m, pi), func=AF.Square),
                             reads=[("x", m, pi)], writes=[("sq", m)])
                    elif follow == "cast":
                        S.op("act", lambda e, m=m: e.activation(out=hT[:, m, :], in_=xs(m, pi), func=AF.Copy),
                             reads=[("x", m, pi)], writes=[("hT", m)])

            def MMK(banks, lhs, rhs_fn, nk, wreads, rhs_reads):
                for k in range(nk):
                    for i, b in enumerate(banks):
                        rd = [rhs_reads[k]] if i == 0 else []
                        if k == 0:
                            rd = rd + [wreads[i]]
                        S.op("pe", lambda e, i=i, b=b, k=k: e.matmul(ps[b][:, 0:TP], lhsT=lhs(i, k), rhs=rhs_fn(k), start=(k == 0), stop=(k == nk - 1)),
                             reads=rd, writes=[("ps", b)], inc=(k == nk - 1))

            def proj_fm(wsrc, ncols_total, col0, nblk_cols, rhs_fn, rhs_reads, nk, consume, kfirst=False):
                nblocks = (nblk_cols + 255) // 256
                mi = 0
                b0 = 0
                if KFIRST and kfirst and nblocks >= 2 and nblk_cols >= 512:
                    wvs = []
                    for bi in range(2):
                        c0 = col0 + bi * 256
                        wvs.append(wload(wsrc[:, c0:c0 + 256].rearrange("(k p) n -> p k n", p=128), v3(nk, 256)))
                    banks = [nb() for _ in range(4)]
                    MMK(banks, lambda i, k: wvs[i // 2][0][:, k, (i % 2) * 128:(i % 2 + 1) * 128], rhs_fn, nk,
                        [wvs[i // 2][1] for i in range(4)], rhs_reads)
                    for b in banks:
                        consume(mi, b)
                        mi += 1
                    b0 = 2
                for bi in range(b0, nblocks):
                    c0 = col0 + bi * 256
                    w = min(256, nblk_cols - bi * 256)
                    wv, wr = wload(wsrc[:, c0:c0 + w].rearrange("(k p) n -> p k n", p=128), v3(nk, w))
                    for mm in range(w // 128):
                        b = nb()
                        MM(b, 0, TP, [(wv[:, k, mm * 128:(mm + 1) * 128], rhs_fn(k)) for k in range(nk)],
                           reads=[wr], preads=[[rhs_reads[k]] for k in range(nk)])
                        consume(mi, b)
                        mi += 1

            S.dma("sp", cst[:], consts_d, writes=["cst"])
            S.dma("sp", vecs[:], vecs_d, writes=["vecs"])
            S.op("act", lambda e: e.activation(out=identb[:], in_=ident, func=AF.Copy), reads=["cst"], writes=["identb"])
            S.op("dve", lambda e: e.memset(onesb[:], 1.0), writes=["onesb"])
            S.op("dve", lambda e: e.tensor_scalar(out=nbf[0:4, :], in0=vecs[0:4, 234:236], scalar1=-1.0, scalar2=None, op0=ALU.mult),
                 reads=["vecs"], writes=["nbf"])
            out_evs = []

            for s in range(NSEQ):
                S.barrier()
                ar = Ar()
                xin = [ar.f32(1024), ar.f32(1024)]
                def load_tile(s, tt):
                    xi = xin[tt % 2]
                    S.dma("sp", xi, x_d[s, tt * 128:(tt + 1) * 128, :], writes=[("xo", tt % 2, 0), ("xo", tt % 2, 1)])
                    for half in range(2):
                        b = nb()
                        for q in range(4):
                            c = half * 4 + q
                            S.op("pe", lambda e, c=c, q=q, b=b, xi=xi: e.transpose(out=ps[b][:, q * 128:(q + 1) * 128],
                                                                                  in_=xi[:, c * 128:(c + 1) * 128], identity=ident),
                                 reads=[("xo", tt % 2, 0), ("xo", tt % 2, 1), "cst"], writes=[("ps", b)], inc=(q == 3))
                        eng = "dve" if half == 0 else "act"
                        dst = xT[:, half * 4:half * 4 + 4, tt * 128:(tt + 1) * 128]
                        src = ps[b][:, :].rearrange("p (q t) -> p q t", q=4)
                        pi_ = (tt * 128) // TP
                        if eng == "dve":
                            S.op("dve", lambda e, dst=dst, src=src: e.tensor_copy(out=dst, in_=src), reads=[("ps", b)],
                                 writes=[("x", half * 4 + q, pi_) for q in range(4)])
                        else:
                            S.op("act", lambda e, dst=dst, src=src: e.activation(out=dst, in_=src, func=AF.Copy), reads=[("ps", b)],
                                 writes=[("x", half * 4 + q, pi_) for q in range(4)])

                for tt in range(NTT):
                    load_tile(s, tt)

                for l in range(NL):
                    j = l // 2
                    even = (l % 2 == 0)
                    S.barrier()
                    if even:
                        S.dma("sp", lbc[:, 0:2560], bce_d[j], writes=["lbc"])
                        S.dma("sp", wstf[:], wst_d[j].rearrange("h s t -> s h t"), writes=["wstf"])
                        for h in range(4):
                            S.op("dve", lambda e, h=h: e.tensor_tensor(out=WT[:, h, :], in0=wstf[:, h, :], in1=triu, op=ALU.mult),
                                 reads=["wstf", "cst"], writes=["WT"])
                    else:
                        S.dma("sp", lbc[:, 0:1024], bco_d[j], writes=["lbc"])
                    def do_pass(s, l, j, even, pi):
                        cols = slice(pi * TP, (pi + 1) * TP)
                        S.dma("pool", pin[:], p_d[l, s, cols, :].rearrange("(c p) f -> p c f", p=128), writes=["pin"])
                        S.barrier()
                        prenorm(0, l, pi)
                        ar = Ar()
                        if even:
                            u = ar.b16(4 * TP).rearrange("p (a t) -> p a t", a=4)
                            vg = [ar.f32(TP), ar.f32(TP)]
                            vn = ar.b16(4 * 512).rearrange("p (c f) -> p c f", c=4)
                            xbuf = ar.f32(4 * 528).rearrange("p (g t) -> p g t", g=4)
                            ptmp = [ar.f32(528), ar.f32(528)]
                            pooled = ar.b16(4 * TP).rearrange("p (g t) -> p g t", g=4)
                            ycat = ar.b16(8 * TP).rearrange("p (a t) -> p a t", a=8)
                            wsrc = ewin[j]
                            def cons_u(mi, b):
                                S.op("act", lambda e, mi=mi, b=b: e.activation(out=u[:, mi, :], in_=ps[b][:], func=AF.Gelu_apprx_tanh),
                                     reads=[("ps", b)], writes=[("u", mi)])
                            proj_fm(wsrc, 1536, 0, 512, lambda k: hT[:, k, :], [("hT", k) for k in range(8)], 8, cons_u, kfirst=True)
                            if pi == 0:
                                S.op("dve", lambda e: e.memset(xbuf[:, :, 0:16], 0.0), writes=[("xbuf", g) for g in range(4)])
                            else:
                                S.op("dve", lambda e: e.tensor_copy(out=xbuf[:, :, 0:16], in_=halo_e[:]), reads=["halo_e"],
                                     writes=[("xbuf", g) for g in range(4)])

                            def cons_xb(mi, b):
                                S.op("act", lambda e, mi=mi, b=b: e.activation(out=xbuf[:, mi, 16:528], in_=ps[b][:], func=AF.Copy),
                                     reads=[("ps", b)], writes=[("xbuf", mi)])
                            proj_fm(wsrc, 1536, 1024, 512, lambda k: hT[:, k, :], [("hT", k) for k in range(8)], 8, cons_xb)
                            S.op("dve", lambda e: e.tensor_copy(out=halo_e[:], in_=xbuf[:, :, 512:528]), reads=[("xbuf", g) for g in range(4)],
                                 writes=["halo_e"])
                            vb = [nb() for _ in range(4)]
                            for bi in range(2):
                                wv, wr = wload(wsrc[:, 512 + bi * 256:512 + (bi + 1) * 256].rearrange("(k p) n -> p k n", p=128), v3(8, 256))
                                for tk in range(4):
                                    MM(vb[tk], bi * 256, (bi + 1) * 256, [(hT[:, k, tk * 128:(tk + 1) * 128], wv[:, k, :]) for k in range(8)],
                                       reads=[wr], preads=[[("hT", k)] for k in range(8)])
                            wpv, wpr = wload(ewpool[j].rearrange("g d e -> d g e"), v3(4, 128))
                            for g in range(4):
                                win = 2 << g
                                src = xbuf[:, g, :]
                                lo = 0
                                for lev in range(g + 1):
                                    sh = 1 << lev
                                    dstt = ptmp[lev % 2]
                                    nlo = lo + sh
                                    S.op("dve", lambda e, src=src, dstt=dstt, nlo=nlo, sh=sh: e.tensor_tensor(
                                        out=dstt[:, nlo:528], in0=src[:, nlo:528], in1=src[:, nlo - sh:528 - sh], op=ALU.add),
                                        reads=[("xbuf", g), ("ptmp", (lev + 1) % 2)], writes=[("ptmp", lev % 2)])
                                    src = dstt
                                    lo = nlo
                                fin = src
                                S.op("dve", lambda e, fin=fin, g=g, win=win: e.scalar_tensor_tensor(
                                    out=pooled[:, g, :], in0=fin[:, 16:528], scalar=1.0 / win, in1=xbuf[:, g, 16:528],
                                    op0=ALU.mult, op1=ALU.subtract),
                                    reads=[("ptmp", g % 2), ("xbuf", g)], writes=[("pooled", g)])
                                if pi == 0:
                                    S.op("dve", lambda e, fin=fin, g=g: e.tensor_tensor(out=tA[:, 0:16], in0=fin[:, 16:32], in1=invcnt[:, g * 16:(g + 1) * 16], op=ALU.mult),
                                         reads=[("ptmp", g % 2), "cst"], writes=["tA"])
                                    S.op("dve", lambda e, g=g: e.tensor_tensor(out=pooled[:, g, 0:16], in0=tA[:, 0:16], in1=xbuf[:, g, 16:32], op=ALU.subtract),
                                         reads=["tA", ("xbuf", g)], writes=[("pooled", g)])
                                b = nb()
                                MM(b, 0, TP, [(wpv[:, g, :], pooled[:, g, :])], reads=[wpr, ("pooled", g)])
                                S.op("act", lambda e, b=b, g=g: e.activation(out=ycat[:, 4 + g, :], in_=ps[b][:], func=AF.Copy,
                                                                             scale=vecs[:, 160 + j * 4 + g:160 + j * 4 + g + 1]),
                                     reads=[("ps", b), "vecs"], writes=[("ycat", 4 + g)])
                            for tk in range(4):
                                b = vb[tk]
                                g_ = vg[tk % 2]
                                S.op("act", lambda e, b=b, g_=g_: e.activation(out=g_, in_=ps[b][:], func=AF.Gelu_apprx_tanh),
                                     reads=[("ps", b)], writes=[("vg", tk % 2)])
                                S.op("act", lambda e, g_=g_, tk=tk: e.activation(out=tA[:], in_=g_, func=AF.Square, accum_out=smal[:, tk:tk + 1]),
                                     reads=[("vg", tk % 2)], writes=["tA", ("smal", tk)])
                                S.op("dve", lambda e, tk=tk: e.tensor_scalar(out=smal[:, tk:tk + 1], in0=smal[:, tk:tk + 1], scalar1=1.0 / 512,
                                                                             scalar2=EPS, op0=ALU.mult, op1=ALU.add),
                                     reads=[("smal", tk)], writes=[("smal", tk)])
                                S.op("dve", lambda e, tk=tk: e.reciprocal(out=smal[:, tk:tk + 1], in_=smal[:, tk:tk + 1]),
                                     reads=[("smal", tk)], writes=[("smal", tk)])
                                S.op("act", lambda e, tk=tk: e.activation(out=smal[:, tk:tk + 1], in_=smal[:, tk:tk + 1], func=AF.Sqrt),
                                     reads=[("smal", tk)], writes=[("smal", tk)])
                                S.op("dve", lambda e, tk=tk, g_=g_: e.scalar_tensor_tensor(out=vn[:, tk, :], in0=g_, scalar=smal[:, tk:tk + 1],
                                                                                           in1=lbc[:, 0:512], op0=ALU.mult, op1=ALU.mult),
                                     reads=[("vg", tk % 2), ("smal", tk), "lbc"], writes=[("vn", tk)])
                            for h in range(4):
                                b = nb()
                                for tk in range(4):
                                    S.op("pe", lambda e, b=b, tk=tk, h=h: e.matmul(ps[b][:, tk * 128:(tk + 1) * 128], lhsT=vn[:, tk, h * 128:(h + 1) * 128],
                                                                                  rhs=WT[:, h, :], start=True, stop=True),
                                         reads=[("vn", tk), "WT"], writes=[("ps", b)], inc=(tk == 3))
                                t = tB[h % 2]
                                S.op("dve", lambda e, b=b, h=h, t=t: e.tensor_tensor(out=t[:], in0=ps[b][:], in1=lbc[:, 512 + h * 512:512 + (h + 1) * 512], op=ALU.add),
                                     reads=[("ps", b), "lbc"], writes=[("tB", h % 2)])
                                S.op("dve", lambda e, h=h, t=t: e.tensor_tensor(out=ycat[:, h, :], in0=t[:], in1=u[:, h, :], op=ALU.mult),
                                     reads=[("tB", h % 2), ("u", h)], writes=[("ycat", h)])
                            def cons_y(mi, b):
                                S.op("act", lambda e, mi=mi, b=b: e.activation(out=ybuf[:, mi, :], in_=ps[b][:], func=AF.Copy),
                                     reads=[("ps", b)], writes=[("ybuf", mi)])
                                ysq(mi, b)
                            proj_fm(ewout[j], 1024, 0, 1024, lambda k: ycat[:, k, :], [("ycat", k) for k in range(8)], 8, cons_y)
                        else:

                            ones4 = cst[0:4, 1344:1472]
                            qk_raw = ar.f32(4 * 516)
                            qkpre = qk_raw.rearrange("p (a t) -> p a t", a=4)
                            DT4 = qk_raw[:, 0:2048].rearrange("p (h t) -> p h t", h=4)
                            qkT = ar.b16(8 * TP).rearrange("p (a t) -> p a t", a=8)
                            ktok = ar.b16(4 * 512).rearrange("p (c f) -> p c f", c=4)
                            vaug = ar.b16(16 * 258).rearrange("p (c h f) -> p c h f", c=4, h=4)
                            sigo = ar.b16(4 * 1024).rearrange("p (c f) -> p c f", c=4)
                            scT4 = ar.b16(4 * 512).rearrange("p (h t) -> p h t", h=4)
                            nd4 = ar.f32(4 * 256).rearrange("p (h f) -> p h f", h=4)
                            kw4 = ar.b16(4 * 128).rearrange("p (h f) -> p h f", h=4)
                            tokq = ar.f32(64)
                            wcb = ar.f32(16)
                            wsrc = owin[j]
                            hTr = [("hT", k) for k in range(8)]

                            def cw(tap, m):
                                cidx = 168 + (j * 4 + tap) * 8 + m
                                return vecs[:, cidx:cidx + 1]
                            def qk_proj(half):
                                if pi == 0:
                                    S.op("dve", lambda e: e.memset(qkpre[:, :, 0:3], 0.0), writes=[("qkpre", a) for a in range(4)])
                                else:
                                    S.op("dve", lambda e, half=half: e.tensor_copy(out=qkpre[:, :, 0:3], in_=halo_o[:, half * 4:half * 4 + 4, 0:3]),
                                         reads=[("halo_o", half)], writes=[("qkpre", a) for a in range(4)])

                                def cons_qk(mi, b):
                                    S.op("act", lambda e, mi=mi, b=b: e.activation(out=qkpre[:, mi, 3:515], in_=ps[b][:], func=AF.Copy),
                                         reads=[("ps", b)], writes=[("qkpre", mi)])
                                proj_fm(wsrc, 3080, half * 512, 512, lambda k: hT[:, k, :], hTr, 8, cons_qk, kfirst=(half == 0))
                                S.op("dve", lambda e, half=half: e.tensor_copy(out=halo_o[:, half * 4:half * 4 + 4, 0:3], in_=qkpre[:, :, 512:515]),
                                     reads=[("qkpre", a) for a in range(4)], writes=[("halo_o", half)])

                            def qk_conv(half):
                                for mi in range(4):
                                    m = half * 4 + mi
                                    acc, accr = ((tA, "tA"), (rstd, "rstd"))[mi % 2]
                                    S.op("dve", lambda e, mi=mi, m=m, acc=acc: e.tensor_scalar(out=acc[:], in0=qkpre[:, mi, 3:515], scalar1=cw(0, m), scalar2=None, op0=ALU.mult),
                                         reads=[("qkpre", mi), "vecs"], writes=[accr])
                                    for tap in range(1, 4):
                                        S.op("dve", lambda e, mi=mi, m=m, tap=tap, acc=acc: e.scalar_tensor_tensor(out=acc[:], in0=qkpre[:, mi, 3 - tap:515 - tap], scalar=cw(tap, m),
                                                                                                         in1=acc[:], op0=ALU.mult, op1=ALU.add),
                                             reads=[("qkpre", mi), accr, "vecs"], writes=[accr])
                                    if half == 1:
                                        S.op("act", lambda e, m=m, acc=acc: e.activation(out=qkT[:, m, :], in_=acc[:], func=AF.Silu), reads=[accr], writes=[("qkT", m)])
                                    else:
                                        t = tB[mi % 2]
                                        S.op("act", lambda e, t=t, acc=acc: e.activation(out=t[:], in_=acc[:], func=AF.Silu), reads=[accr], writes=[("tB", mi % 2)])
                                        S.op("dve", lambda e, t=t, m=m: e.tensor_scalar(out=qkT[:, m, :], in0=t[:], scalar1=float(128 ** -0.5), scalar2=None, op0=ALU.mult),
                                             reads=[("tB", mi % 2)], writes=[("qkT", m)])

                            def k_transposes():
                                for h in range(4):
                                    b = nb()
                                    for c in range(4):
                                        S.op("pe", lambda e, b=b, c=c, h=h: e.transpose(out=psb[b][:, c * 128:(c + 1) * 128], in_=qkT[:, 4 + h, c * 128:(c + 1) * 128], identity=identb[:]),
                                             reads=[("qkT", 4 + h), "identb"], writes=[("ps", b)], inc=(c == 3))
                                    S.op("dve", lambda e, b=b, h=h: e.tensor_copy(out=ktok[:, :, h * 128:(h + 1) * 128], in_=psb[b][:, 0:512].rearrange("p (c f) -> p c f", c=4)),
                                         reads=[("ps", b)], writes=[("ktok", h)])

                            def v_proj():
                                S.op("dve", lambda e: e.memset(vaug[:, :, :, 256:258], 1.0), writes=["vones"])
                                for hh in range(4):
                                    wv, wr = wload(wsrc[:, 1024 + hh * 256:1024 + (hh + 1) * 256].rearrange("(k p) n -> p k n", p=128), v3(8, 256))
                                    for tk in range(4):
                                        b = nb()
                                        MM(b, 0, 256, [(hT[:, k, tk * 128:(tk + 1) * 128], wv[:, k, :]) for k in range(8)], reads=[wr],
                                           preads=[[("hT", k)] for k in range(8)])
                                        S.op("act", lambda e, b=b, tk=tk, hh=hh: e.activation(out=vaug[:, tk, hh, 0:256], in_=ps[b][:, 0:256], func=AF.Copy),
                                             reads=[("ps", b)], writes=[("vaug", tk, hh)])

                            def o_proj():
                                for hh in range(4):
                                    wv, wr = wload(wsrc[:, 2048 + hh * 256:2048 + (hh + 1) * 256].rearrange("(k p) n -> p k n", p=128), v3(8, 256))
                                    for tk in range(4):
                                        b = nb()
                                        MM(b, 0, 256, [(hT[:, k, tk * 128:(tk + 1) * 128], wv[:, k, :]) for k in range(8)], reads=[wr],
                                           preads=[[("hT", k)] for k in range(8)])
                                        t = tB[tk % 2]
                                        S.op("act", lambda e, b=b, t=t: e.activation(out=t[:, 0:256], in_=ps[b][:, 0:256], func=AF.Sigmoid),
                                             reads=[("ps", b)], writes=[("tB", tk % 2)])
                                        S.op("dve", lambda e, t=t, tk=tk, hh=hh: e.tensor_tensor(out=sigo[:, tk, hh * 256:(hh + 1) * 256], in0=t[:, 0:256],
                                                                                             in1=lbc[:, hh * 256:(hh + 1) * 256], op=ALU.mult),
                                             reads=[("tB", tk % 2), "lbc"], writes=[("sigo", tk, hh)])

                            G = lambda i: ybuf[0:4, i, :]
                            Gr = lambda i: ("ybuf", i)
                            gbank = []

                            def gates_mm():
                                wg, wgr = wload(wsrc[:, 3072:3080].rearrange("(k p) n -> p k n", p=128), v3(8, 8))
                                bA = nb()
                                MM(bA, 0, 512, [(wg[:, k, 0:4], hT[:, k, :]) for k in range(8)], reads=[wgr] + hTr, rows=4)
                                bB = nb()
                                MM(bB, 0, 512, [(wg[:, k, 4:8], hT[:, k, :]) for k in range(8)], reads=[wgr] + hTr, rows=4)
                                gbank.extend([bA, bB])

                            def gates_part1():
                                bA, bB = gbank
                                S.op("dve", lambda e: e.tensor_scalar(out=G(0), in0=ps[bA][0:4, :], scalar1=vecs[0:4, 232 + j:233 + j], scalar2=None, op0=ALU.add),
                                     reads=[("ps", bA), "vecs"], writes=[Gr(0)])
                                S.op("act", lambda e: e.activation(out=G(1), in_=ps[bB][0:4, :], func=AF.Exp, scale=-1.0, bias=nbf[0:4, j:j + 1]),
                                     reads=[("ps", bB), "nbf"], writes=[Gr(1)])
                                S.op("act", lambda e: e.activation(out=G(1), in_=G(1), func=AF.Ln, bias=1.0), reads=[Gr(1)], writes=[Gr(1)])
                                for c in range(4):
                                    cs = slice(c * 128, (c + 1) * 128)
                                    S.op("dve", lambda e, cs=cs: e.tensor_tensor_scan(out=G(2)[:, cs], data0=ones4, data1=G(1)[:, cs], initial=0.0, op0=ALU.mult, op1=ALU.add),
                                         reads=[Gr(1), "cst"], writes=[Gr(2)])
                                S.op("dve", lambda e: e.tensor_tensor(out=G(3), in0=G(0), in1=G(2), op=ALU.add), reads=[Gr(0), Gr(2)], writes=[Gr(3)])
                                if pi == 0:
                                    S.op("dve", lambda e: e.memset(mprev[0:4, 0:1], 0.0), writes=["mprev"])
                                else:
                                    S.op("dve", lambda e: e.tensor_copy(out=mprev[0:4, 0:1], in_=mprev[0:4, 4:5]), reads=["mprev"], writes=["mprev"])

                            def gates_part2():
                                for c in range(4):
                                    cs = slice(c * 128, (c + 1) * 128)
                                    S.op("dve", lambda e, cs=cs, c=c: e.tensor_tensor_scan(out=G(4)[:, cs], data0=ones4, data1=G(3)[:, cs], initial=mprev[0:4, c:c + 1],
                                                                                        op0=ALU.mult, op1=ALU.max),
                                         reads=[Gr(3), "cst", "mprev"], writes=[Gr(4)])
                                    S.op("dve", lambda e, c=c: e.tensor_tensor(out=mprev[0:4, c + 1:c + 2], in0=G(4)[:, c * 128 + 127:c * 128 + 128],
                                                                              in1=G(2)[:, c * 128 + 127:c * 128 + 128], op=ALU.subtract),
                                         reads=[Gr(4), Gr(2), "mprev"], writes=["mprev"])
                                    S.op("dve", lambda e, c=c: e.tensor_scalar(out=smal[0:4, 8 + c:9 + c], in0=G(4)[:, c * 128 + 127:c * 128 + 128], scalar1=-1.0, scalar2=None, op0=ALU.mult),
                                         reads=[Gr(4)], writes=[("smal", 8)])

                            def gates_part3():
                                for c in range(4):
                                    cs = slice(c * 128, (c + 1) * 128)
                                    S.op("act", lambda e, cs=cs, c=c: e.activation(out=G(5)[:, cs], in_=G(4)[:, cs], func=AF.Exp, scale=-1.0, bias=mprev[0:4, c:c + 1]),
                                         reads=[Gr(4), "mprev"], writes=[Gr(5)])
                                    S.op("act", lambda e, cs=cs, c=c: e.activation(out=G(7)[:, cs], in_=G(3)[:, cs], func=AF.Exp, bias=smal[0:4, 8 + c:9 + c]),
                                         reads=[Gr(3), ("smal", 8)], writes=[Gr(7)])
                                S.op("dve", lambda e: e.tensor_tensor(out=G(6), in0=G(2), in1=G(4), op=ALU.subtract), reads=[Gr(2), Gr(4)], writes=[Gr(6)])
                                S.op("act", lambda e: e.activation(out=G(6), in_=G(6), func=AF.Exp), reads=[Gr(6)], writes=[Gr(6)])
                                S.op("dve", lambda e: e.tensor_tensor(out=smal[0:4, 12:16], in0=mprev[0:4, 0:4], in1=smal[0:4, 8:12], op=ALU.add),
                                     reads=["mprev", ("smal", 8)], writes=[("smal", 12)])
                                S.op("act", lambda e: e.activation(out=smal[0:4, 12:16], in_=smal[0:4, 12:16], func=AF.Exp), reads=[("smal", 12)], writes=[("smal", 12)])
                                S.op("dve", lambda e: e.tensor_scalar(out=G(4), in0=G(4), scalar1=-1.0, scalar2=None, op0=ALU.mult), reads=[Gr(4)], writes=[Gr(4)])
                                bt = nb()
                                for qi, gi in enumerate((3, 5, 6, 7)):
                                    for c in range(4):
                                        col = (qi * 4 + c) * 4
                                        S.op("pe", lambda e, gi=gi, c=c, col=col: e.transpose(out=ps[bt][:, col:col + 4], in_=ybuf[0:4, gi, c * 128:(c + 1) * 128], identity=cst[0:4, 0:4]),
                                             reads=[Gr(gi), "cst"], writes=[("ps", bt)], inc=(qi == 3 and c == 3))
                                S.op("dve", lambda e: e.tensor_copy(out=tokq, in_=ps[bt][:, 0:64]), reads=[("ps", bt)], writes=["tokq"])
                                bw = nb()
                                for h in range(4):
                                    S.op("pe", lambda e, h=h: e.matmul(ps[bw][:, h * 4:(h + 1) * 4], lhsT=cst[0:4, 768 + h * 128:768 + (h + 1) * 128], rhs=smal[0:4, 12:16], start=True, stop=True),
                                         reads=[("smal", 12), "cst"], writes=[("ps", bw)], inc=(h == 3))
                                S.op("dve", lambda e: e.tensor_copy(out=wcb, in_=ps[bw][:, 0:16]), reads=[("ps", bw)], writes=["wcb"])
                                if pi == 0:
                                    S.op("dve", lambda e: e.memset(Cst[:], 0.0), writes=[("Cst", h) for h in range(4)])
                                    S.op("dve", lambda e: e.memset(Cb[:], 0.0), writes=[("Cb", h) for h in range(4)])

                            gates_mm()
                            gates_part1()
                            qk_proj(0)
                            v_proj()
                            qk_conv(0)
                            gates_part2()
                            qk_proj(1)
                            o_proj()
                            qk_conv(1)
                            gates_part3()
                            k_transposes()
                            QK4 = [("qkpre", a) for a in range(4)]
                            for h in range(4):
                                bS = nb()
                                for c in range(4):
                                    cs = slice(c * 128, (c + 1) * 128)
                                    S.op("pe", lambda e, cs=cs, h=h, bS=bS: e.matmul(ps[bS][:, cs], lhsT=qkT[:, 4 + h, cs], rhs=qkT[:, h, cs], start=True, stop=True),
                                         reads=[("qkT", 4 + h), ("qkT", h)], writes=[("ps", bS)], inc=(c == 3))
                                bM = nb()
                                S.op("pe", lambda e, h=h, bM=bM: e.matmul(ps[bM][:, :], lhsT=cst[0:4, 768 + h * 128:768 + (h + 1) * 128], rhs=G(4), start=True, stop=False),
                                     reads=[Gr(4), "cst"], writes=[("ps", bM)], inc=False)
                                S.op("pe", lambda e, bM=bM: e.matmul(ps[bM][:, :], lhsT=ident, rhs=maskneg4, start=False, stop=True),
                                     reads=["cst"], writes=[("ps", bM)])
                                for c in range(4):
                                    cs = slice(c * 128, (c + 1) * 128)
                                    S.op("act", lambda e, cs=cs, c=c, h=h, bM=bM: e.activation(out=DT4[:, h, cs], in_=ps[bM][:, cs], func=AF.Exp, bias=tokq[:, c * 4 + h:c * 4 + h + 1]),
                                         reads=[("ps", bM), "tokq"], writes=[("DT", h)] + (QK4 if c == 0 else []))
                                S.op("dve", lambda e, bS=bS, h=h: e.tensor_tensor(out=scT4[:, h, :], in0=ps[bS][:, :], in1=DT4[:, h, :], op=ALU.mult),
                                     reads=[("ps", bS), ("DT", h)], writes=[("scT", h)])
                            PB = [0, 1, 2, 3]
                            UB = [4, 5, 6, 7]
                            DB = 4
                            den_ps = ps[DB][:, 300:308]
                            den8 = smal[:, 24:32].rearrange("p (h t) -> p h t", t=2)
                            d1 = smal[:, 32:36]
                            d2 = smal[:, 36:40]
                            rden4 = smal[:, 40:44]
                            ssn4 = smal[:, 44:48]
                            vv = smal[:, 48:52]
                            r4 = smal[:, 52:56]
                            for c in range(4):
                                cs = slice(c * 128, (c + 1) * 128)
                                wi4 = tokq[:, 16 + c * 4:16 + c * 4 + 4]
                                fl4 = tokq[:, 32 + c * 4:32 + c * 4 + 4]
                                for h in range(4):
                                    S.op("act", lambda e, c=c, h=h: e.activation(out=kw4[:, h, :], in_=ktok[:, c, h * 128:(h + 1) * 128], func=AF.Copy,
                                                                                 scale=tokq[:, 48 + c * 4 + h:48 + c * 4 + h + 1]),
                                         reads=[("ktok", h), "tokq"], writes=[("kw", h)])
                                for h in range(4):
                                    b = PB[h]
                                    S.op("pe", lambda e, b=b, h=h, cs=cs: e.matmul(ps[b][:, 0:256], lhsT=qkT[:, h, cs], rhs=Cb[:, h, 0:256], start=True, stop=True),
                                         reads=[("qkT", h), ("Cb", h)], writes=[("ps", b)], inc=False)
                                    S.op("pe", lambda e, b=b, h=h, cs=cs, c=c: e.matmul(ps[b][:, 256:512], lhsT=scT4[:, h, cs], rhs=vaug[:, c, h, 0:256], start=True, stop=True),
                                         reads=[("scT", h), ("vaug", c, h)], writes=[("ps", b)])
                                for h in range(4):
                                    S.op("pe", lambda e, h=h, cs=cs: e.matmul(ps[DB][:, 300 + 2 * h:301 + 2 * h], lhsT=qkT[:, h, cs], rhs=Cb[:, h, 256:257], start=True, stop=True),
                                         reads=[("qkT", h), ("Cb", h)], writes=[("ps", DB)], inc=False)
                                    S.op("pe", lambda e, h=h, cs=cs, c=c: e.matmul(ps[DB][:, 301 + 2 * h:302 + 2 * h], lhsT=scT4[:, h, cs], rhs=vaug[:, c, h, 256:257], start=True, stop=True),
                                         reads=[("scT", h), "vones"], writes=[("ps", DB)], inc=(h == 3))
                                S.op("act", lambda e: e.activation(out=smal[:, 24:32], in_=den_ps, func=AF.Copy), reads=[("ps", DB)], writes=[("smal", 24)])
                                for h in range(4):
                                    MM(UB[h], 0, 257, [(kw4[:, h, :], vaug[:, c, h, 0:257])], reads=[("kw", h), ("vaug", c, h), "vones"])
                                for h in range(4):
                                    b = PB[h]
                                    S.op("act", lambda e, b=b, h=h: e.activation(out=nd4[:, h, :], in_=ps[b][:, 256:512], func=AF.Copy), reads=[("ps", b)], writes=[("nd", h)])
                                    S.op("dve", lambda e, b=b, h=h, c=c: e.scalar_tensor_tensor(out=nd4[:, h, :], in0=ps[b][:, 0:256], scalar=tokq[:, 16 + c * 4 + h:16 + c * 4 + h + 1],
                                                                                             in1=nd4[:, h, :], op0=ALU.mult, op1=ALU.add),
                                         reads=[("ps", b), ("nd", h), "tokq"], writes=[("nd", h)])
                                for h in range(4):
                                    bU = UB[h]
                                    S.op("dve", lambda e, h=h, c=c, bU=bU: e.scalar_tensor_tensor(out=Cst[:, h, :], in0=Cst[:, h, :], scalar=wcb[:, h * 4 + c:h * 4 + c + 1],
                                                                                               in1=ps[bU][:, 0:257], op0=ALU.mult, op1=ALU.add),
                                         reads=[("Cst", h), "wcb", ("ps", bU)], writes=[("Cst", h)])
                                for h in range(4):
                                    S.op("act", lambda e, h=h: e.activation(out=tA[:, 0:256], in_=nd4[:, h, :], func=AF.Square, accum_out=ssn4[:, h:h + 1]),
                                         reads=[("nd", h)], writes=[("ssn", h)])
                                for h in range(4):
                                    S.op("act", lambda e, h=h: e.activation(out=Cb[:, h, 0:257], in_=Cst[:, h, :], func=AF.Copy), reads=[("Cst", h)], writes=[("Cb", h)])
                                S.op("dve", lambda e: e.tensor_tensor(out=d1, in0=den8[:, :, 0], in1=wi4, op=ALU.mult), reads=[("smal", 24), "tokq"], writes=[("smal", 32)])
                                S.op("dve", lambda e: e.tensor_tensor(out=d1, in0=d1, in1=den8[:, :, 1], op=ALU.add), reads=[("smal", 24), ("smal", 32)], writes=[("smal", 32)])
                                S.op("dve", lambda e: e.tensor_scalar(out=d2, in0=d1, scalar1=-1.0, scalar2=None, op0=ALU.mult), reads=[("smal", 32)], writes=[("smal", 36)])
                                S.op("dve", lambda e: e.tensor_tensor(out=d1, in0=d1, in1=d2, op=ALU.max), reads=[("smal", 32), ("smal", 36)], writes=[("smal", 32)])
                                S.op("dve", lambda e: e.tensor_tensor(out=d1, in0=d1, in1=fl4, op=ALU.max), reads=[("smal", 32), "tokq"], writes=[("smal", 32)])
                                S.op("dve", lambda e: e.reciprocal(out=rden4, in_=d1), reads=[("smal", 32)], writes=[("smal", 40)])
                                S.op("dve", lambda e: e.tensor_tensor(out=vv, in0=rden4, in1=rden4, op=ALU.mult), reads=[("smal", 40)], writes=[("smal", 48)])
                                S.op("dve", lambda e: e.tensor_tensor(out=vv, in0=vv, in1=ssn4, op=ALU.mult), reads=[("smal", 48)] + [("ssn", h) for h in range(4)], writes=[("smal", 48)])
                                S.op("dve", lambda e: e.tensor_scalar(out=vv, in0=vv, scalar1=1.0 / 256, scalar2=EPS, op0=ALU.mult, op1=ALU.add), reads=[("smal", 48)], writes=[("smal", 48)])
                                S.op("dve", lambda e: e.reciprocal(out=vv, in_=vv), reads=[("smal", 48)], writes=[("smal", 48)])
                                S.op("act", lambda e: e.activation(out=vv, in_=vv, func=AF.Sqrt), reads=[("smal", 48)], writes=[("smal", 48)])
                                S.op("dve", lambda e: e.tensor_tensor(out=r4, in0=vv, in1=rden4, op=ALU.mult), reads=[("smal", 48), ("smal", 40)], writes=[("smal", 52)])
                                for h in range(4):
                                    hs = slice(h * 256, (h + 1) * 256)
                                    S.op("dve", lambda e, c=c, h=h, hs=hs: e.scalar_tensor_tensor(out=sigo[:, c, hs], in0=nd4[:, h, :], scalar=smal[:, 52 + h:53 + h], in1=sigo[:, c, hs],
                                                                                               op0=ALU.mult, op1=ALU.mult),
                                         reads=[("nd", h), ("smal", 52), ("sigo", c, h)], writes=[("sigo", c, h)])
                            for c in range(4):
                                for half in range(2):
                                    b = nb()
                                    for q in range(4):
                                        ee = half * 4 + q
                                        S.op("pe", lambda e, b=b, q=q, ee=ee, c=c: e.transpose(out=psb[b][:, q * 128:(q + 1) * 128], in_=sigo[:, c, ee * 128:(ee + 1) * 128], identity=identb[:]),
                                             reads=[("sigo", c, ee // 2), "identb"], writes=[("ps", b)], inc=(q == 3))
                                    S.op("dve", lambda e, b=b, c=c, half=half: e.tensor_copy(out=hT[:, half * 4:half * 4 + 4, c * 128:(c + 1) * 128],
                                                                                          in_=psb[b][:, 0:512].rearrange("p (q t) -> p q t", q=4)),
                                         reads=[("ps", b)], writes=[("hT", half * 4 + q) for q in range(4)])
                            def cons_yo(mi, b):
                                S.op("act", lambda e, mi=mi, b=b: e.activation(out=ybuf[:, mi, :], in_=ps[b][:], func=AF.Copy),
                                     reads=[("ps", b)], writes=[("ybuf", mi)])
                                ysq(mi, b)
                            proj_fm(owout[j], 1024, 0, 1024, lambda k: hT[:, k, :], hTr, 8, cons_yo)
                        postnorm_add(1, l, pi, follow="sq")
                        S.barrier()
                        prenorm(2, l, pi, presq=True)
                        ar = Ar()
                        hid = ar.b16(22 * TP).rearrange("p (a t) -> p a t", a=22)
                        sg = [ar.f32(TP), ar.f32(TP)]
                        for j0 in range(0, 22, 2):
                            nj = 2
                            gv, gr = wload(wgu[l][:, j0 * 128:(j0 + nj) * 128].rearrange("(k p) n -> p k n", p=128), v3(8, nj * 128))
                            uv, ur = wload(wgu[l][:, 2816 + j0 * 128:2816 + (j0 + nj) * 128].rearrange("(k p) n -> p k n", p=128), v3(8, nj * 128))
                            hTk = [("hT", k) for k in range(8)]
                            bgs = [nb() for _ in range(4)]
                            if KFIRST and j0 == 0:
                                MMK(bgs, lambda i, k: (gv if i % 2 == 0 else uv)[:, k, (i // 2) * 128:(i // 2 + 1) * 128], lambda k: hT[:, k, :], 8,
                                    [gr, ur, gr, ur], hTk)
                            else:
                                for i, b in enumerate(bgs):
                                    wv_, wr_ = (gv, gr) if i % 2 == 0 else (uv, ur)
                                    MM(b, 0, TP, [(wv_[:, k, (i // 2) * 128:(i // 2 + 1) * 128], hT[:, k, :]) for k in range(8)],
                                       reads=[wr_], preads=[[hTk[k]] for k in range(8)])
                            for jj in range(nj):
                                jx = j0 + jj
                                bg, bu = bgs[2 * jj], bgs[2 * jj + 1]
                                sgt = sg[jx % 2]
                                S.op("act", lambda e, bg=bg, sgt=sgt: e.activation(out=sgt, in_=ps[bg][:], func=AF.Silu),
                                     reads=[("ps", bg)], writes=[("sg", jx % 2)])
                                S.op("dve", lambda e, bu=bu, sgt=sgt, jx=jx: e.tensor_tensor(out=hid[:, jx, :], in0=ps[bu][:], in1=sgt, op=ALU.mult),
                                     reads=[("ps", bu), ("sg", jx % 2)], writes=[("hid", jx)])
                        for m in range(8):
                            dvs = []
                            for jh in range(2):
                                dv, dr = wload(wdn[l][jh * 1408:(jh + 1) * 1408, m * 128:(m + 1) * 128].rearrange("(j p) n -> p j n", p=128), v3(11, 128))
                                dvs.append((dv, dr))
                            b = nb()
                            MM(b, 0, TP, [(dvs[jx // 11][0][:, jx % 11, :], hid[:, jx, :]) for jx in range(22)], reads=[],
                               preads=[[dvs[jx // 11][1], ("hid", jx)] for jx in range(22)])
                            S.op("act", lambda e, m=m, b=b: e.activation(out=ybuf[:, m, :], in_=ps[b][:], func=AF.Copy),
                                 reads=[("ps", b)], writes=[("ybuf", m)])
                            ysq(m, b)
                        postnorm_add(3, l, pi, follow="cast")
                        ar = Ar()
                        ar.off = 6656
                        sgp = [ar.f32(TP), ar.f32(TP)]
                        for kk in range(2):
                            b = nb()
                            for c in range(4):
                                S.op("pe", lambda e, b=b, c=c, kk=kk: e.transpose(out=psb[b][:, c * 128:(c + 1) * 128], in_=pin[:, c, kk * 128:(kk + 1) * 128],
                                                                                 identity=identb[:]),
                                     reads=["pin", "identb"], writes=[("ps", b)], inc=(c == 3))
                            S.op("dve", lambda e, b=b, kk=kk: e.tensor_copy(out=pT[:, kk, :], in_=psb[b][:, 0:512]), reads=[("ps", b)], writes=[("pT", kk)])
                        prv, prr = wload(pproj[l].rearrange("(k p) n -> p k n", p=128), v3(2, 1024))
                        for mi in range(8):
                            b2 = nb()
                            MM(b2, 0, TP, [(prv[:, k, mi * 128:(mi + 1) * 128], pT[:, k, :]) for k in range(2)],
                               reads=[prr, ("pT", 0), ("pT", 1)])
                            S.op("act", lambda e, b2=b2, mi=mi: e.activation(out=ybuf[:, mi, :], in_=ps[b2][:], func=AF.Copy),
                                 reads=[("ps", b2)], writes=[("ybuf", mi)])

                        def cons_e(mi, b):
                            sgt = sgp[mi % 2]
                            S.op("act", lambda e, b=b, sgt=sgt: e.activation(out=sgt, in_=ps[b][:], func=AF.Sigmoid),
                                 reads=[("ps", b)], writes=[("sgp", mi % 2)])
                            S.op("dve", lambda e, sgt=sgt, mi=mi: e.tensor_tensor(out=ybuf[:, mi, :], in0=ybuf[:, mi, :], in1=sgt, op=ALU.mult),
                                 reads=[("ybuf", mi), ("sgp", mi % 2)], writes=[("ybuf", mi)])
                            S.op("act", lambda e, mi=mi: e.activation(out=sq[:, mi, :], in_=ybuf[:, mi, :], func=AF.Square),
                                 reads=[("ybuf", mi)], writes=[("sq", mi)])
                        proj_fm(pgate[l], 1024, 0, 1024, lambda k: hT[:, k, :], [("hT", k) for k in range(8)], 8, cons_e, kfirst=True)
                        postnorm_add(4, l, pi)

                    for pi in range(NPASS):
                        do_pass(s, l, j, even, pi)

                S.barrier()
                ar = Ar()
                xo = [ar.f32(1024), ar.f32(1024)]
                def store_tile(s, tt):
                    xi = xo[tt % 2]
                    pi_ = (tt * 128) // TP
                    for half in range(2):
                        b = nb()
                        for q in range(4):
                            c = half * 4 + q
                            S.op("pe", lambda e, c=c, q=q, b=b: e.transpose(out=ps[b][:, q * 128:(q + 1) * 128],
                                                                          in_=xT[:, c, tt * 128:(tt + 1) * 128], identity=ident),
                                 reads=[("x", c, pi_), "cst"], writes=[("ps", b)], inc=(q == 3))
                        if half == 0:
                            S.op("dve", lambda e, b=b, xi=xi: e.tensor_copy(out=xi[:, 0:512], in_=ps[b][:]), reads=[("ps", b)],
                                 writes=[("xo", tt % 2, 0)])
                        else:
                            S.op("act", lambda e, b=b, xi=xi: e.activation(out=xi[:, 512:1024], in_=ps[b][:], func=AF.Copy), reads=[("ps", b)],
                                 writes=[("xo", tt % 2, 1)])
                    out_evs.append(S.dma("sp", out_d[s, tt * 128:(tt + 1) * 128, :], xi, reads=[("xo", tt % 2, 0), ("xo", tt % 2, 1)]))

                for tt in range(NTT):
                    store_tile(s, tt)

            S.finish("sp", out_evs)
            return wlist

        S0 = Sched(nc, sems)
        plan = record(S0, None)
        S = Sched(nc, sems)
        record(S, plan)
        S.emit()
    return nc


def _consts():
    c = np.zeros((128, NCONST), np.float32)
    c[:, 0:128] = np.eye(128, dtype=np.float32)
    s = np.arange(128)[:, None]
    t = np.arange(128)[None, :]
    tri = (s <= t).astype(np.float32)
    c[:, 128:256] = tri
    c[:, 256:768] = np.tile(np.where(s <= t, 0.0, NEGBIG).astype(np.float32), (1, 4))
    for h in range(4):
        c[h, 768 + h * 128:768 + (h + 1) * 128] = 1.0
    for g, win in enumerate((2, 4, 8, 16)):
        c[:, 1280 + g * 16:1280 + (g + 1) * 16] = (1.0 / np.minimum(np.arange(16) + 1, win)).astype(np.float32)[None, :]
    c[:, 1344:1472] = 1.0
    return c


def _prep_shared(inp):
    f = lambda a: np.ascontiguousarray(np.asarray(a, dtype=np.float32))
    vecs = np.zeros((128, NV), np.float32)
    kinds = ["mix_pre_gain", "mix_post_gain", "ffn_pre_gain", "ffn_post_gain", "ple_post_gain"]
    for k, name in enumerate(kinds):
        g = f(inp[name])
        vecs[:, k * 32:(k + 1) * 32] = g.reshape(4, 8, 128).transpose(2, 0, 1).reshape(128, 32)
    vecs[:, 160:168] = f(inp["even_b_scale"]).reshape(2, 4, 128).transpose(2, 0, 1).reshape(128, 8)
    vecs[:, 168:232] = f(inp["odd_conv_w"]).reshape(2, 4, 8, 128).transpose(3, 0, 1, 2).reshape(128, 64)
    vecs[0:4, 232:234] = f(inp["odd_b_i"]).T
    vecs[0:4, 234:236] = f(inp["odd_b_f"]).T
    bce = np.zeros((2, 128, 2560), np.float32)
    bce[:, :, 0:512] = f(inp["even_a_v_gain"])[:, None, :]
    bs = f(inp["even_a_bs"])
    bce[:, :, 512:2560] = np.tile(bs[:, :, None, :], (1, 1, 4, 1)).reshape(2, 1, 2048)
    bco = np.ascontiguousarray(np.broadcast_to(f(inp["odd_h_gain"])[:, None, :], (2, 128, 1024)))
    d = {
        "even_w_in": f(inp["even_w_in"]), "even_w_out": f(inp["even_w_out"]), "even_b_wpool": f(inp["even_b_wpool"]),
        "ws_t": np.ascontiguousarray(f(inp["even_a_ws"]).transpose(0, 1, 3, 2)),
        "odd_w_in": f(inp["odd_w_in"]), "odd_w_out": f(inp["odd_w_out"]),
        "ffn_w_gate_up": f(inp["ffn_w_gate_up"]), "ffn_w_down": f(inp["ffn_w_down"]),
        "ple_proj": f(inp["ple_proj"]), "ple_gate": f(inp["ple_gate"]),
        "vecs": vecs, "bc_even": bce, "bc_odd": bco, "consts": _consts(),
    }
    return d


def run(inp, n_cores, NSEQ, T, NL, trace=False):
    x = np.asarray(inp["x"], dtype=np.float32)
    p = np.asarray(inp["p"], dtype=np.float32)
    shared = _prep_shared(inp)
    nc = build_nc(NSEQ, T, NL)
    in_maps = []
    for c in range(n_cores):
        m = dict(shared)
        m["x"] = np.ascontiguousarray(x[c * NSEQ:(c + 1) * NSEQ])
        m["p"] = np.ascontiguousarray(p[:, c * NSEQ:(c + 1) * NSEQ])
        in_maps.append(m)
    res = run_bass_kernel_spmd(nc, in_maps, core_ids=list(range(n_cores)), trace=trace)
    out = np.concatenate([r["out"] for r in res.results], axis=0)
    return out, res


def kernel(**inputs):
    out, _ = run(inputs, 8, 2, 2048, 4)
    return out.astype(np.float32)
```

```python
import numpy as np
from contextlib import ExitStack
import concourse.bass as bass
import concourse.mybir as mybir
from concourse.bass_utils import run_bass_kernel_spmd

F32 = mybir.dt.float32
BF16 = mybir.dt.bfloat16
ALU = mybir.AluOpType
AF = mybir.ActivationFunctionType

ENGS = ("pe", "act", "dve", "pool", "sp")
NDMASEM = 8
EPS = 1e-6
NV = 236
NCONST = 1472
TP = 512
NSLOT = 6
WLOOK = 3
WSZ = 2048
NEGBIG = -30000.0
KFIRST = False


class Sched:
    def __init__(self, nc, sems):
        self.nc = nc
        self.ops = {e: [] for e in ENGS}
        self.sem, self.dsem = sems
        self.cnt = {e: 0 for e in ENGS}
        self.dcnt = {q: 0 for q in ("sp", "pool")}
        self.seen = {}
        self.last_w = {}
        self.readers = {}

    def _semh(self, k):
        return self.sem[k] if isinstance(k, str) else self.dsem[k[0]][k[1]]

    def _need(self, eng, ev, waits):
        if ev is None:
            return
        k, v = ev
        if k == "pe" and eng == "pe":
            return
        if self.seen.get((eng, k), 0) >= v:
            return
        if isinstance(k, str):
            assert v <= self.cnt[k], ("wait on un-incremented event", eng, k, v, self.cnt[k])
        self.seen[(eng, k)] = v
        waits.append((k, v))

    def _deps(self, eng, reads, writes):
        waits = []
        for r in reads:
            self._need(eng, self.last_w.get(r), waits)
        for w in writes:
            self._need(eng, self.last_w.get(w), waits)
            for ev in self.readers.get(w, ()):
                self._need(eng, ev, waits)
        return waits

    def _commit(self, ev, reads, writes):
        for r in reads:
            self.readers.setdefault(r, []).append(ev)
        for w in writes:
            self.last_w[w] = ev
            self.readers[w] = []

    def op(self, eng, fn, reads=(), writes=(), inc=True):
        waits = self._deps(eng, reads, writes)
        ev = (eng, self.cnt[eng] + 1)
        if inc:
            self.cnt[eng] += 1
        self.ops[eng].append((waits, fn, (eng, 1) if inc else None))
        self._commit(ev, reads, writes)
        return ev

    def dma(self, q, out, in_, reads=(), writes=()):
        i = self.dcnt[q]
        self.dcnt[q] += 1
        slot = i % NDMASEM
        k = (q, slot)
        waits = self._deps(q, reads, writes)
        prev = i // NDMASEM
        if prev > 0:
            self._need(q, (k, 16 * prev), waits)
        ev = (k, 16 * (prev + 1))
        self.ops[q].append((waits, lambda e: e.dma_start(out=out, in_=in_), (k, 16)))
        self._commit(ev, reads, writes)
        return ev

    def barrier(self, engs=("pe", "act", "dve", "pool")):
        for e in engs:
            waits = []
            for k in engs:
                if k != e and self.cnt[k] > 0:
                    self._need(e, (k, self.cnt[k]), waits)
            if waits:
                self.ops[e].append((waits, None, None))

    def finish(self, eng, evs):
        waits = []
        for ev in evs:
            self._need(eng, ev, waits)
        self.ops[eng].append((waits, None, None))

    def emit(self):
        nc = self.nc
        engmap = {"pe": "tensor", "act": "scalar", "dve": "vector", "pool": "gpsimd", "sp": "sync"}
        with nc.Block() as block:
            for e in ENGS:
                ops = self.ops[e]

                def body(engine, ops=ops):
                    for waits, fn, inc in ops:
                        for k, v in waits:
                            engine.wait_ge(self._semh(k), v)
                        if fn is None:
                            continue
                        ins = fn(engine)
                        if inc is not None:
                            ins.then_inc(self._semh(inc[0]), inc[1])

                getattr(block, engmap[e])(body)


def build_nc(NSEQ=2, T=2048, NL=4):
    nc = bass.Bass("TRN2", target_bir_lowering=False)
    D = 1024
    NPASS = T // TP
    NTT = T // 128

    def din(name, shape):
        return nc.dram_tensor(name, list(shape), F32, kind="ExternalInput").ap()

    x_d = din("x", [NSEQ, T, D])
    p_d = din("p", [4, NSEQ, T, 256])
    ewin = din("even_w_in", [2, 1024, 1536])
    ewout = din("even_w_out", [2, 1024, 1024])
    ewpool = din("even_b_wpool", [2, 4, 128, 128])
    wst_d = din("ws_t", [2, 4, 128, 128])
    owin = din("odd_w_in", [2, 1024, 3080])
    owout = din("odd_w_out", [2, 1024, 1024])
    wgu = din("ffn_w_gate_up", [4, 1024, 5632])
    wdn = din("ffn_w_down", [4, 2816, 1024])
    pproj = din("ple_proj", [4, 256, 1024])
    pgate = din("ple_gate", [4, 1024, 1024])
    vecs_d = din("vecs", [128, NV])
    bce_d = din("bc_even", [2, 128, 2560])
    bco_d = din("bc_odd", [2, 128, 1024])
    consts_d = din("consts", [128, NCONST])
    out_d = nc.dram_tensor("out", [NSEQ, T, D], F32, kind="ExternalOutput").ap()

    with ExitStack() as st:
        sems = ({e: st.enter_context(nc.semaphore("s_" + e)) for e in ENGS},
                {q: [st.enter_context(nc.semaphore("d_%s%d" % (q, i))) for i in range(NDMASEM)] for q in ("sp", "pool")})

        def sb(n, sh, dt):
            return st.enter_context(nc.sbuf_tensor("sb_" + n, list(sh), dt))

        xT = sb("xT", [128, 8, T], F32)
        cst = sb("cst", [128, NCONST], F32)
        vecs = sb("vecs", [128, NV], F32)
        nbf = sb("nbf", [128, 2], F32)
        lbc = sb("lbc", [128, 2560], F32)
        identb = sb("identb", [128, 128], BF16)
        onesb = sb("onesb", [128, 128], BF16)
        hT = sb("hT", [128, 8, TP], BF16)
        ybuf = sb("ybuf", [128, 8, TP], F32)
        sq = sb("sq", [128, 8, TP], BF16)
        rstd = sb("rstd", [128, TP], F32)
        tA = sb("tA", [128, TP], F32)
        tB = [sb("tB%d" % i, [128, TP], F32) for i in range(2)]
        wsl = [sb("wsl%d" % i, [128, WSZ], BF16) for i in range(NSLOT)]
        pin = sb("pin", [128, 4, 256], BF16)
        pT = sb("pT", [128, 2, TP], BF16)
        WT = sb("WT", [128, 4, 128], BF16)
        wstf = sb("wstf", [128, 4, 128], F32)
        halo_e = sb("halo_e", [128, 4, 16], F32)
        halo_o = sb("halo_o", [128, 8, 4], F32)
        Cst = sb("Cst", [128, 4, 257], F32)
        Cb = sb("Cb", [128, 4, 258], BF16)
        mprev = sb("mprev", [4, 8], F32)
        smal = sb("smal", [128, 64], F32)
        NA = 12288
        arena = sb("arena", [128, NA], F32)
        ps = [st.enter_context(nc.psum_tensor("ps%d" % i, [128, 512], F32)) for i in range(8)]
        psb = [p_[:].bitcast(BF16) for p_ in ps]
        def record(S, plan):
            bank = [0]

            def nb():
                b = bank[0]
                bank[0] = (b + 1) % 8
                return b

            class Ar:
                def __init__(self):
                    self.off = 0

                def f32(self, n):
                    a = arena[:, self.off:self.off + n]
                    self.off += n
                    assert self.off <= NA, self.off
                    return a

                def b16(self, n):
                    w = (n + 1) // 2
                    a = arena[:, self.off:self.off + w].bitcast(BF16)
                    self.off += w
                    assert self.off <= NA, self.off
                    return a

            ident = cst[:, 0:128]
            triu = cst[:, 128:256]
            maskneg4 = cst[:, 256:768]
            invcnt = cst[:, 1280:1344]

            def gcol(kind, l, c):
                return vecs[:, (kind * 4 + l) * 8 + c:(kind * 4 + l) * 8 + c + 1]

            def MM(b, lo, hi, pairs, reads, rows=128, preads=None):
                n = len(pairs)
                for i, (l, r) in enumerate(pairs):
                    rd = list(reads) if i == 0 else []
                    if preads is not None:
                        rd += list(preads[i])
                    S.op("pe", lambda e, l=l, r=r, i=i: e.matmul(ps[b][0:rows, lo:hi], lhsT=l, rhs=r,
                                                                  start=(i == 0), stop=(i == n - 1)),
                         reads=rd, writes=[("ps", b)], inc=(i == n - 1))

            wcount = [0]
            wissued = [0]
            wlist = []

            def wload(src_ap, view):
                i = wcount[0]
                wcount[0] += 1
                wlist.append((src_ap, view))
                if plan is None:
                    S.dma("pool", view(wsl[i % NSLOT]), src_ap, writes=[("w", i % NSLOT)])
                else:
                    while wissued[0] < min(i + WLOOK + 1, len(plan)):
                        n = wissued[0]
                        wissued[0] += 1
                        src_n, view_n = plan[n]
                        S.dma("pool", view_n(wsl[n % NSLOT]), src_n, writes=[("w", n % NSLOT)])
                return view(wsl[i % NSLOT]), ("w", i % NSLOT)

            def v3(k, n):
                return lambda s_: s_[:, 0:k * n].rearrange("p (k n) -> p k n", k=k)

            def norm_rstd(srcs, regs, Dn, presq=False):
                nch = len(srcs)
                if not presq:
                    for c in range(nch):
                        if c % 2 == 0:
                            S.op("act", lambda e, c=c: e.activation(out=sq[:, c, :], in_=srcs[c], func=AF.Square),
                                 reads=[regs[c]], writes=[("sq", c)])
                        else:
                            S.op("dve", lambda e, c=c: e.tensor_tensor(out=sq[:, c, :], in0=srcs[c], in1=srcs[c], op=ALU.mult),
                                 reads=[regs[c]], writes=[("sq", c)])
                b = nb()
                MM(b, 0, TP, [(onesb[:], sq[:, c, :]) for c in range(nch)], reads=["onesb"],
                   preads=[[("sq", c)] for c in range(nch)])
                S.op("act", lambda e: e.activation(out=tA[:], in_=ps[b][:], func=AF.Ln, scale=1.0 / Dn, bias=EPS),
                     reads=[("ps", b)], writes=["tA"])
                S.op("act", lambda e: e.activation(out=rstd[:], in_=tA[:], func=AF.Exp, scale=-0.5), reads=["tA"], writes=["rstd"])

            def xs(c, pi):
                return xT[:, c, pi * TP:(pi + 1) * TP]

            def prenorm(kind, l, pi, presq=False):
                norm_rstd([xs(c, pi) for c in range(8)], [("x", c, pi) for c in range(8)], D, presq=presq)
                for c in range(8):
                    S.op("dve", lambda e, c=c: e.scalar_tensor_tensor(out=hT[:, c, :], in0=xs(c, pi), scalar=gcol(kind, l, c),
                                                                      in1=rstd[:], op0=ALU.mult, op1=ALU.mult),
                         reads=[("x", c, pi), "rstd", "vecs"], writes=[("hT", c)])

            def ysq(mi, b):
                S.op("dve", lambda e, mi=mi, b=b: e.tensor_tensor(out=sq[:, mi, :], in0=ps[b][:], in1=ybuf[:, mi, :], op=ALU.mult),
                     reads=[("ps", b), ("ybuf", mi)], writes=[("sq", mi)])

            def postnorm_add(kind, l, pi, follow=None):
                norm_rstd([ybuf[:, m, :] for m in range(8)], [("ybuf", m) for m in range(8)], D, presq=True)
                for m in range(8):
                    t = tB[m % 2]
                    S.op("dve", lambda e, m=m, t=t: e.scalar_tensor_tensor(out=t[:], in0=ybuf[:, m, :], scalar=gcol(kind, l, m),
                                                                           in1=rstd[:], op0=ALU.mult, op1=ALU.mult),
                         reads=[("ybuf", m), "rstd", "vecs"], writes=[("tB", m % 2)])
                    S.op("dve", lambda e, m=m, t=t: e.tensor_tensor(out=xs(m, pi), in0=xs(m, pi), in1=t[:], op=ALU.add),
                         reads=[("tB", m % 2), ("x", m, pi)], writes=[("x", m, pi)])
                    if follow == "sq":
                        S.op("act", lambda e, m=m: e.activation(out=sq[:, m, :], in_=xs(m, pi), func=AF.Square),
                             reads=[("x", m, pi)], writes=[("sq", m)])
                    elif follow == "cast":
                        S.op("act", lambda e, m=m: e.activation(out=hT[:, m, :], in_=xs(m, pi), func=AF.Copy),
                             reads=[("x", m, pi)], writes=[("hT", m)])

            def MMK(banks, lhs, rhs_fn, nk, wreads, rhs_reads):
                for k in range(nk):
                    for i, b in enumerate(banks):
                        rd = [rhs_reads[k]] if i == 0 else []
                        if k == 0:
                            rd = rd + [wreads[i]]
                        S.op("pe", lambda e, i=i, b=b, k=k: e.matmul(ps[b][:, 0:TP], lhsT=lhs(i, k), rhs=rhs_fn(k), start=(k == 0), stop=(k == nk - 1)),
                             reads=rd, writes=[("ps", b)], inc=(k == nk - 1))

            def proj_fm(wsrc, ncols_total, col0, nblk_cols, rhs_fn, rhs_reads, nk, consume, kfirst=False):
                nblocks = (nblk_cols + 255) // 256
                mi = 0
                b0 = 0
                if KFIRST and kfirst and nblocks >= 2 and nblk_cols >= 512:
                    wvs = []
                    for bi in range(2):
                        c0 = col0 + bi * 256
                        wvs.append(wload(wsrc[:, c0:c0 + 256].rearrange("(k p) n -> p k n", p=128), v3(nk, 256)))
                    banks = [nb() for _ in range(4)]
                    MMK(banks, lambda i, k: wvs[i // 2][0][:, k, (i % 2) * 128:(i % 2 + 1) * 128], rhs_fn, nk,
                        [wvs[i // 2][1] for i in range(4)], rhs_reads)
                    for b in banks:
                        consume(mi, b)
                        mi += 1
                    b0 = 2
                for bi in range(b0, nblocks):
                    c0 = col0 + bi * 256
                    w = min(256, nblk_cols - bi * 256)
                    wv, wr = wload(wsrc[:, c0:c0 + w].rearrange("(k p) n -> p k n", p=128), v3(nk, w))
                    for mm in range(w // 128):
                        b = nb()
                        MM(b, 0, TP, [(wv[:, k, mm * 128:(mm + 1) * 128], rhs_fn(k)) for k in range(nk)],
                           reads=[wr], preads=[[rhs_reads[k]] for k in range(nk)])
                        consume(mi, b)
                        mi += 1

            S.dma("sp", cst[:], consts_d, writes=["cst"])
            S.dma("sp", vecs[:], vecs_d, writes=["vecs"])
            S.op("act", lambda e: e.activation(out=identb[:], in_=ident, func=AF.Copy), reads=["cst"], writes=["identb"])
            S.op("dve", lambda e: e.memset(onesb[:], 1.0), writes=["onesb"])
            S.op("dve", lambda e: e.tensor_scalar(out=nbf[0:4, :], in0=vecs[0:4, 234:236], scalar1=-1.0, scalar2=None, op0=ALU.mult),
                 reads=["vecs"], writes=["nbf"])
            out_evs = []

            for s in range(NSEQ):
                S.barrier()
                ar = Ar()
                xin = [ar.f32(1024), ar.f32(1024)]
                def load_tile(s, tt):
                    xi = xin[tt % 2]
                    S.dma("sp", xi, x_d[s, tt * 128:(tt + 1) * 128, :], writes=[("xo", tt % 2, 0), ("xo", tt % 2, 1)])
                    for half in range(2):
                        b = nb()
                        for q in range(4):
                            c = half * 4 + q
                            S.op("pe", lambda e, c=c, q=q, b=b, xi=xi: e.transpose(out=ps[b][:, q * 128:(q + 1) * 128],
                                                                                  in_=xi[:, c * 128:(c + 1) * 128], identity=ident),
                                 reads=[("xo", tt % 2, 0), ("xo", tt % 2, 1), "cst"], writes=[("ps", b)], inc=(q == 3))
                        eng = "dve" if half == 0 else "act"
                        dst = xT[:, half * 4:half * 4 + 4, tt * 128:(tt + 1) * 128]
                        src = ps[b][:, :].rearrange("p (q t) -> p q t", q=4)
                        pi_ = (tt * 128) // TP
                        if eng == "dve":
                            S.op("dve", lambda e, dst=dst, src=src: e.tensor_copy(out=dst, in_=src), reads=[("ps", b)],
                                 writes=[("x", half * 4 + q, pi_) for q in range(4)])
                        else:
                            S.op("act", lambda e, dst=dst, src=src: e.activation(out=dst, in_=src, func=AF.Copy), reads=[("ps", b)],
                                 writes=[("x", half * 4 + q, pi_) for q in range(4)])

                for tt in range(NTT):
                    load_tile(s, tt)

                for l in range(NL):
                    j = l // 2
                    even = (l % 2 == 0)
                    S.barrier()
                    if even:
                        S.dma("sp", lbc[:, 0:2560], bce_d[j], writes=["lbc"])
                        S.dma("sp", wstf[:], wst_d[j].rearrange("h s t -> s h t"), writes=["wstf"])
                        for h in range(4):
                            S.op("dve", lambda e, h=h: e.tensor_tensor(out=WT[:, h, :], in0=wstf[:, h, :], in1=triu, op=ALU.mult),
                                 reads=["wstf", "cst"], writes=["WT"])
                    else:
                        S.dma("sp", lbc[:, 0:1024], bco_d[j], writes=["lbc"])
                    def do_pass(s, l, j, even, pi):
                        cols = slice(pi * TP, (pi + 1) * TP)
                        S.dma("pool", pin[:], p_d[l, s, cols, :].rearrange("(c p) f -> p c f", p=128), writes=["pin"])
                        S.barrier()
                        prenorm(0, l, pi)
                        ar = Ar()
                        if even:
                            u = ar.b16(4 * TP).rearrange("p (a t) -> p a t", a=4)
                            vg = [ar.f32(TP), ar.f32(TP)]
                            vn = ar.b16(4 * 512).rearrange("p (c f) -> p c f", c=4)
                            xbuf = ar.f32(4 * 528).rearrange("p (g t) -> p g t", g=4)
                            ptmp = [ar.f32(528), ar.f32(528)]
                            pooled = ar.b16(4 * TP).rearrange("p (g t) -> p g t", g=4)
                            ycat = ar.b16(8 * TP).rearrange("p (a t) -> p a t", a=8)
                            wsrc = ewin[j]
                            def cons_u(mi, b):
                                S.op("act", lambda e, mi=mi, b=b: e.activation(out=u[:, mi, :], in_=ps[b][:], func=AF.Gelu_apprx_tanh),
                                     reads=[("ps", b)], writes=[("u", mi)])
                            proj_fm(wsrc, 1536, 0, 512, lambda k: hT[:, k, :], [("hT", k) for k in range(8)], 8, cons_u, kfirst=True)
                            if pi == 0:
                                S.op("dve", lambda e: e.memset(xbuf[:, :, 0:16], 0.0), writes=[("xbuf", g) for g in range(4)])
                            else:
                                S.op("dve", lambda e: e.tensor_copy(out=xbuf[:, :, 0:16], in_=halo_e[:]), reads=["halo_e"],
                                     writes=[("xbuf", g) for g in range(4)])

                            def cons_xb(mi, b):
                                S.op("act", lambda e, mi=mi, b=b: e.activation(out=xbuf[:, mi, 16:528], in_=ps[b][:], func=AF.Copy),
                                     reads=[("ps", b)], writes=[("xbuf", mi)])
                            proj_fm(wsrc, 1536, 1024, 512, lambda k: hT[:, k, :], [("hT", k) for k in range(8)], 8, cons_xb)
                            S.op("dve", lambda e: e.tensor_copy(out=halo_e[:], in_=xbuf[:, :, 512:528]), reads=[("xbuf", g) for g in range(4)],
                                 writes=["halo_e"])
                            vb = [nb() for _ in range(4)]
                            for bi in range(2):
                                wv, wr = wload(wsrc[:, 512 + bi * 256:512 + (bi + 1) * 256].rearrange("(k p) n -> p k n", p=128), v3(8, 256))
                                for tk in range(4):
                                    MM(vb[tk], bi * 256, (bi + 1) * 256, [(hT[:, k, tk * 128:(tk + 1) * 128], wv[:, k, :]) for k in range(8)],
                                       reads=[wr], preads=[[("hT", k)] for k in range(8)])
                            wpv, wpr = wload(ewpool[j].rearrange("g d e -> d g e"), v3(4, 128))
                            for g in range(4):
                                win = 2 << g
                                src = xbuf[:, g, :]
                                lo = 0
                                for lev in range(g + 1):
                                    sh = 1 << lev
                                    dstt = ptmp[lev % 2]
                                    nlo = lo + sh
                                    S.op("dve", lambda e, src=src, dstt=dstt, nlo=nlo, sh=sh: e.tensor_tensor(
                                        out=dstt[:, nlo:528], in0=src[:, nlo:528], in1=src[:, nlo - sh:528 - sh], op=ALU.add),
                                        reads=[("xbuf", g), ("ptmp", (lev + 1) % 2)], writes=[("ptmp", lev % 2)])
                                    src = dstt
                                    lo = nlo
                                fin = src
                                S.op("dve", lambda e, fin=fin, g=g, win=win: e.scalar_tensor_tensor(
                                    out=pooled[:, g, :], in0=fin[:, 16:528], scalar=1.0 / win, in1=xbuf[:, g, 16:528],
                                    op0=ALU.mult, op1=ALU.subtract),
                                    reads=[("ptmp", g % 2), ("xbuf", g)], writes=[("pooled", g)])
                                if pi == 0:
                                    S.op("dve", lambda e, fin=fin, g=g: e.tensor_tensor(out=tA[:, 0:16], in0=fin[:, 16:32], in1=invcnt[:, g * 16:(g + 1) * 16], op=ALU.mult),
                                         reads=[("ptmp", g % 2), "cst"], writes=["tA"])
                                    S.op("dve", lambda e, g=g: e.tensor_tensor(out=pooled[:, g, 0:16], in0=tA[:, 0:16], in1=xbuf[:, g, 16:32], op=ALU.subtract),
                                         reads=["tA", ("xbuf", g)], writes=[("pooled", g)])
                                b = nb()
                                MM(b, 0, TP, [(wpv[:, g, :], pooled[:, g, :])], reads=[wpr, ("pooled", g)])
                                S.op("act", lambda e, b=b, g=g: e.activation(out=ycat[:, 4 + g, :], in_=ps[b][:], func=AF.Copy,
                                                                             scale=vecs[:, 160 + j * 4 + g:160 + j * 4 + g + 1]),
                                     reads=[("ps", b), "vecs"], writes=[("ycat", 4 + g)])
                            for tk in range(4):
                                b = vb[tk]
                                g_ = vg[tk % 2]
                                S.op("act", lambda e, b=b, g_=g_: e.activation(out=g_, in_=ps[b][:], func=AF.Gelu_apprx_tanh),
                                     reads=[("ps", b)], writes=[("vg", tk % 2)])
                                S.op("act", lambda e, g_=g_, tk=tk: e.activation(out=tA[:], in_=g_, func=AF.Square, accum_out=smal[:, tk:tk + 1]),
                                     reads=[("vg", tk % 2)], writes=["tA", ("smal", tk)])
                                S.op("dve", lambda e, tk=tk: e.tensor_scalar(out=smal[:, tk:tk + 1], in0=smal[:, tk:tk + 1], scalar1=1.0 / 512,
                                                                             scalar2=EPS, op0=ALU.mult, op1=ALU.add),
                                     reads=[("smal", tk)], writes=[("smal", tk)])
                                S.op("dve", lambda e, tk=tk: e.reciprocal(out=smal[:, tk:tk + 1], in_=smal[:, tk:tk + 1]),
                                     reads=[("smal", tk)], writes=[("smal", tk)])
                                S.op("act", lambda e, tk=tk: e.activation(out=smal[:, tk:tk + 1], in_=smal[:, tk:tk + 1], func=AF.Sqrt),
                                     reads=[("smal", tk)], writes=[("smal", tk)])
                                S.op("dve", lambda e, tk=tk, g_=g_: e.scalar_tensor_tensor(out=vn[:, tk, :], in0=g_, scalar=smal[:, tk:tk + 1],
                                                                                           in1=lbc[:, 0:512], op0=ALU.mult, op1=ALU.mult),
                                     reads=[("vg", tk % 2), ("smal", tk), "lbc"], writes=[("vn", tk)])
                            for h in range(4):
                                b = nb()
                                for tk in range(4):
                                    S.op("pe", lambda e, b=b, tk=tk, h=h: e.matmul(ps[b][:, tk * 128:(tk + 1) * 128], lhsT=vn[:, tk, h * 128:(h + 1) * 128],
                                                                                  rhs=WT[:, h, :], start=True, stop=True),
                                         reads=[("vn", tk), "WT"], writes=[("ps", b)], inc=(tk == 3))
                                t = tB[h % 2]
                                S.op("dve", lambda e, b=b, h=h, t=t: e.tensor_tensor(out=t[:], in0=ps[b][:], in1=lbc[:, 512 + h * 512:512 + (h + 1) * 512], op=ALU.add),
                                     reads=[("ps", b), "lbc"], writes=[("tB", h % 2)])
                                S.op("dve", lambda e, h=h, t=t: e.tensor_tensor(out=ycat[:, h, :], in0=t[:], in1=u[:, h, :], op=ALU.mult),
                                     reads=[("tB", h % 2), ("u", h)], writes=[("ycat", h)])
                            def cons_y(mi, b):
                                S.op("act", lambda e, mi=mi, b=b: e.activation(out=ybuf[:, mi, :], in_=ps[b][:], func=AF.Copy),
                                     reads=[("ps", b)], writes=[("ybuf", mi)])
                                ysq(mi, b)
                            proj_fm(ewout[j], 1024, 0, 1024, lambda k: ycat[:, k, :], [("ycat", k) for k in range(8)], 8, cons_y)
                        else:

                            ones4 = cst[0:4, 1344:1472]
                            qk_raw = ar.f32(4 * 516)
                            qkpre = qk_raw.rearrange("p (a t) -> p a t", a=4)
                            DT4 = qk_raw[:, 0:2048].rearrange("p (h t) -> p h t", h=4)
                            qkT = ar.b16(8 * TP).rearrange("p (a t) -> p a t", a=8)
                            ktok = ar.b16(4 * 512).rearrange("p (c f) -> p c f", c=4)
                            vaug = ar.b16(16 * 258).rearrange("p (c h f) -> p c h f", c=4, h=4)
                            sigo = ar.b16(4 * 1024).rearrange("p (c f) -> p c f", c=4)
                            scT4 = ar.b16(4 * 512).rearrange("p (h t) -> p h t", h=4)
                            nd4 = ar.f32(4 * 256).rearrange("p (h f) -> p h f", h=4)
                            kw4 = ar.b16(4 * 128).rearrange("p (h f) -> p h f", h=4)
                            tokq = ar.f32(64)
                            wcb = ar.f32(16)
                            wsrc = owin[j]
                            hTr = [("hT", k) for k in range(8)]

                            def cw(tap, m):
                                cidx = 168 + (j * 4 + tap) * 8 + m
                                return vecs[:, cidx:cidx + 1]
                            def qk_proj(half):
                                if pi == 0:
                                    S.op("dve", lambda e: e.memset(qkpre[:, :, 0:3], 0.0), writes=[("qkpre", a) for a in range(4)])
                                else:
                                    S.op("dve", lambda e, half=half: e.tensor_copy(out=qkpre[:, :, 0:3], in_=halo_o[:, half * 4:half * 4 + 4, 0:3]),
                                         reads=[("halo_o", half)], writes=[("qkpre", a) for a in range(4)])

                                def cons_qk(mi, b):
                                    S.op("act", lambda e, mi=mi, b=b: e.activation(out=qkpre[:, mi, 3:515], in_=ps[b][:], func=AF.Copy),
                                         reads=[("ps", b)], writes=[("qkpre", mi)])
                                proj_fm(wsrc, 3080, half * 512, 512, lambda k: hT[:, k, :], hTr, 8, cons_qk, kfirst=(half == 0))
                                S.op("dve", lambda e, half=half: e.tensor_copy(out=halo_o[:, half * 4:half * 4 + 4, 0:3], in_=qkpre[:, :, 512:515]),
                                     reads=[("qkpre", a) for a in range(4)], writes=[("halo_o", half)])

                            def qk_conv(half):
                                for mi in range(4):
                                    m = half * 4 + mi
                                    acc, accr = ((tA, "tA"), (rstd, "rstd"))[mi % 2]
                                    S.op("dve", lambda e, mi=mi, m=m, acc=acc: e.tensor_scalar(out=acc[:], in0=qkpre[:, mi, 3:515], scalar1=cw(0, m), scalar2=None, op0=ALU.mult),
                                         reads=[("qkpre", mi), "vecs"], writes=[accr])
                                    for tap in range(1, 4):
                                        S.op("dve", lambda e, mi=mi, m=m, tap=tap, acc=acc: e.scalar_tensor_tensor(out=acc[:], in0=qkpre[:, mi, 3 - tap:515 - tap], scalar=cw(tap, m),
                                                                                                         in1=acc[:], op0=ALU.mult, op1=ALU.add),
                                             reads=[("qkpre", mi), accr, "vecs"], writes=[accr])
                                    if half == 1:
                                        S.op("act", lambda e, m=m, acc=acc: e.activation(out=qkT[:, m, :], in_=acc[:], func=AF.Silu), reads=[accr], writes=[("qkT", m)])
                                    else:
                                        t = tB[mi % 2]
                                        S.op("act", lambda e, t=t, acc=acc: e.activation(out=t[:], in_=acc[:], func=AF.Silu), reads=[accr], writes=[("tB", mi % 2)])
                                        S.op("dve", lambda e, t=t, m=m: e.tensor_scalar(out=qkT[:, m, :], in0=t[:], scalar1=float(128 ** -0.5), scalar2=None, op0=ALU.mult),
                                             reads=[("tB", mi % 2)], writes=[("qkT", m)])

                            def k_transposes():
                                for h in range(4):
                                    b = nb()
                                    for c in range(4):
                                        S.op("pe", lambda e, b=b, c=c, h=h: e.transpose(out=psb[b][:, c * 128:(c + 1) * 128], in_=qkT[:, 4 + h, c * 128:(c + 1) * 128], identity=identb[:]),
                                             reads=[("qkT", 4 + h), "identb"], writes=[("ps", b)], inc=(c == 3))
                                    S.op("dve", lambda e, b=b, h=h: e.tensor_copy(out=ktok[:, :, h * 128:(h + 1) * 128], in_=psb[b][:, 0:512].rearrange("p (c f) -> p c f", c=4)),
                                         reads=[("ps", b)], writes=[("ktok", h)])

                            def v_proj():
                                S.op("dve", lambda e: e.memset(vaug[:, :, :, 256:258], 1.0), writes=["vones"])
                                for hh in range(4):
                                    wv, wr = wload(wsrc[:, 1024 + hh * 256:1024 + (hh + 1) * 256].rearrange("(k p) n -> p k n", p=128), v3(8, 256))
                                    for tk in range(4):
                                        b = nb()
                                        MM(b, 0, 256, [(hT[:, k, tk * 128:(tk + 1) * 128], wv[:, k, :]) for k in range(8)], reads=[wr],
                                           preads=[[("hT", k)] for k in range(8)])
                                        S.op("act", lambda e, b=b, tk=tk, hh=hh: e.activation(out=vaug[:, tk, hh, 0:256], in_=ps[b][:, 0:256], func=AF.Copy),
                                             reads=[("ps", b)], writes=[("vaug", tk, hh)])

                            def o_proj():
                                for hh in range(4):
                                    wv, wr = wload(wsrc[:, 2048 + hh * 256:2048 + (hh + 1) * 256].rearrange("(k p) n -> p k n", p=128), v3(8, 256))
                                    for tk in range(4):
                                        b = nb()
                                        MM(b, 0, 256, [(hT[:, k, tk * 128:(tk + 1) * 128], wv[:, k, :]) for k in range(8)], reads=[wr],
                                           preads=[[("hT", k)] for k in range(8)])
                                        t = tB[tk % 2]
                                        S.op("act", lambda e, b=b, t=t: e.activation(out=t[:, 0:256], in_=ps[b][:, 0:256], func=AF.Sigmoid),
                                             reads=[("ps", b)], writes=[("tB", tk % 2)])
                                        S.op("dve", lambda e, t=t, tk=tk, hh=hh: e.tensor_tensor(out=sigo[:, tk, hh * 256:(hh + 1) * 256], in0=t[:, 0:256],
                                                                                             in1=lbc[:, hh * 256:(hh + 1) * 256], op=ALU.mult),
                                             reads=[("tB", tk % 2), "lbc"], writes=[("sigo", tk, hh)])

                            G = lambda i: ybuf[0:4, i, :]
                            Gr = lambda i: ("ybuf", i)
                            gbank = []

                            def gates_mm():
                                wg, wgr = wload(wsrc[:, 3072:3080].rearrange("(k p) n -> p k n", p=128), v3(8, 8))
                                bA = nb()
                                MM(bA, 0, 512, [(wg[:, k, 0:4], hT[:, k, :]) for k in range(8)], reads=[wgr] + hTr, rows=4)
                                bB = nb()
                                MM(bB, 0, 512, [(wg[:, k, 4:8], hT[:, k, :]) for k in range(8)], reads=[wgr] + hTr, rows=4)
                                gbank.extend([bA, bB])

                            def gates_part1():
                                bA, bB = gbank
                                S.op("dve", lambda e: e.tensor_scalar(out=G(0), in0=ps[bA][0:4, :], scalar1=vecs[0:4, 232 + j:233 + j], scalar2=None, op0=ALU.add),
                                     reads=[("ps", bA), "vecs"], writes=[Gr(0)])
                                S.op("act", lambda e: e.activation(out=G(1), in_=ps[bB][0:4, :], func=AF.Exp, scale=-1.0, bias=nbf[0:4, j:j + 1]),
                                     reads=[("ps", bB), "nbf"], writes=[Gr(1)])
                                S.op("act", lambda e: e.activation(out=G(1), in_=G(1), func=AF.Ln, bias=1.0), reads=[Gr(1)], writes=[Gr(1)])
                                for c in range(4):
                                    cs = slice(c * 128, (c + 1) * 128)
                                    S.op("dve", lambda e, cs=cs: e.tensor_tensor_scan(out=G(2)[:, cs], data0=ones4, data1=G(1)[:, cs], initial=0.0, op0=ALU.mult, op1=ALU.add),
                                         reads=[Gr(1), "cst"], writes=[Gr(2)])
                                S.op("dve", lambda e: e.tensor_tensor(out=G(3), in0=G(0), in1=G(2), op=ALU.add), reads=[Gr(0), Gr(2)], writes=[Gr(3)])
                                if pi == 0:
                                    S.op("dve", lambda e: e.memset(mprev[0:4, 0:1], 0.0), writes=["mprev"])
                                else:
                                    S.op("dve", lambda e: e.tensor_copy(out=mprev[0:4, 0:1], in_=mprev[0:4, 4:5]), reads=["mprev"], writes=["mprev"])

                            def gates_part2():
                                for c in range(4):
                                    cs = slice(c * 128, (c + 1) * 128)
                                    S.op("dve", lambda e, cs=cs, c=c: e.tensor_tensor_scan(out=G(4)[:, cs], data0=ones4, data1=G(3)[:, cs], initial=mprev[0:4, c:c + 1],
                                                                                        op0=ALU.mult, op1=ALU.max),
                                         reads=[Gr(3), "cst", "mprev"], writes=[Gr(4)])
                                    S.op("dve", lambda e, c=c: e.tensor_tensor(out=mprev[0:4, c + 1:c + 2], in0=G(4)[:, c * 128 + 127:c * 128 + 128],
                                                                              in1=G(2)[:, c * 128 + 127:c * 128 + 128], op=ALU.subtract),
                                         reads=[Gr(4), Gr(2), "mprev"], writes=["mprev"])
                                    S.op("dve", lambda e, c=c: e.tensor_scalar(out=smal[0:4, 8 + c:9 + c], in0=G(4)[:, c * 128 + 127:c * 128 + 128], scalar1=-1.0, scalar2=None, op0=ALU.mult),
                                         reads=[Gr(4)], writes=[("smal", 8)])

                            def gates_part3():
                                for c in range(4):
                                    cs = slice(c * 128, (c + 1) * 128)
                                    S.op("act", lambda e, cs=cs, c=c: e.activation(out=G(5)[:, cs], in_=G(4)[:, cs], func=AF.Exp, scale=-1.0, bias=mprev[0:4, c:c + 1]),
                                         reads=[Gr(4), "mprev"], writes=[Gr(5)])
                                    S.op("act", lambda e, cs=cs, c=c: e.activation(out=G(7)[:, cs], in_=G(3)[:, cs], func=AF.Exp, bias=smal[0:4, 8 + c:9 + c]),
                                         reads=[Gr(3), ("smal", 8)], writes=[Gr(7)])
                                S.op("dve", lambda e: e.tensor_tensor(out=G(6), in0=G(2), in1=G(4), op=ALU.subtract), reads=[Gr(2), Gr(4)], writes=[Gr(6)])
                                S.op("act", lambda e: e.activation(out=G(6), in_=G(6), func=AF.Exp), reads=[Gr(6)], writes=[Gr(6)])
                                S.op("dve", lambda e: e.tensor_tensor(out=smal[0:4, 12:16], in0=mprev[0:4, 0:4], in1=smal[0:4, 8:12], op=ALU.add),
                                     reads=["mprev", ("smal", 8)], writes=[("smal", 12)])
                                S.op("act", lambda e: e.activation(out=smal[0:4, 12:16], in_=smal[0:4, 12:16], func=AF.Exp), reads=[("smal", 12)], writes=[("smal", 12)])
                                S.op("dve", lambda e: e.tensor_scalar(out=G(4), in0=G(4), scalar1=-1.0, scalar2=None, op0=ALU.mult), reads=[Gr(4)], writes=[Gr(4)])
                                bt = nb()
                                for qi, gi in enumerate((3, 5, 6, 7)):
                                    for c in range(4):
                                        col = (qi * 4 + c) * 4
                                        S.op("pe", lambda e, gi=gi, c=c, col=col: e.transpose(out=ps[bt][:, col:col + 4], in_=ybuf[0:4, gi, c * 128:(c + 1) * 128], identity=cst[0:4, 0:4]),
                                             reads=[Gr(gi), "cst"], writes=[("ps", bt)], inc=(qi == 3 and c == 3))
                                S.op("dve", lambda e: e.tensor_copy(out=tokq, in_=ps[bt][:, 0:64]), reads=[("ps", bt)], writes=["tokq"])
                                bw = nb()
                                for h in range(4):
                                    S.op("pe", lambda e, h=h: e.matmul(ps[bw][:, h * 4:(h + 1) * 4], lhsT=cst[0:4, 768 + h * 128:768 + (h + 1) * 128], rhs=smal[0:4, 12:16], start=True, stop=True),
                                         reads=[("smal", 12), "cst"], writes=[("ps", bw)], inc=(h == 3))
                                S.op("dve", lambda e: e.tensor_copy(out=wcb, in_=ps[bw][:, 0:16]), reads=[("ps", bw)], writes=["wcb"])
                                if pi == 0:
                                    S.op("dve", lambda e: e.memset(Cst[:], 0.0), writes=[("Cst", h) for h in range(4)])
                                    S.op("dve", lambda e: e.memset(Cb[:], 0.0), writes=[("Cb", h) for h in range(4)])

                            gates_mm()
                            gates_part1()
                            qk_proj(0)
                            v_proj()
                            qk_conv(0)
                            gates_part2()
                            qk_proj(1)
                            o_proj()
                            qk_conv(1)
                            gates_part3()
                            k_transposes()
                            QK4 = [("qkpre", a) for a in range(4)]
                            for h in range(4):
                                bS = nb()
                                for c in range(4):
                                    cs = slice(c * 128, (c + 1) * 128)
                                    S.op("pe", lambda e, cs=cs, h=h, bS=bS: e.matmul(ps[bS][:, cs], lhsT=qkT[:, 4 + h, cs], rhs=qkT[:, h, cs], start=True, stop=True),
                                         reads=[("qkT", 4 + h), ("qkT", h)], writes=[("ps", bS)], inc=(c == 3))
                                bM = nb()
                                S.op("pe", lambda e, h=h, bM=bM: e.matmul(ps[bM][:, :], lhsT=cst[0:4, 768 + h * 128:768 + (h + 1) * 128], rhs=G(4), start=True, stop=False),
                                     reads=[Gr(4), "cst"], writes=[("ps", bM)], inc=False)
                                S.op("pe", lambda e, bM=bM: e.matmul(ps[bM][:, :], lhsT=ident, rhs=maskneg4, start=False, stop=True),
                                     reads=["cst"], writes=[("ps", bM)])
                                for c in range(4):
                                    cs = slice(c * 128, (c + 1) * 128)
                                    S.op("act", lambda e, cs=cs, c=c, h=h, bM=bM: e.activation(out=DT4[:, h, cs], in_=ps[bM][:, cs], func=AF.Exp, bias=tokq[:, c * 4 + h:c * 4 + h + 1]),
                                         reads=[("ps", bM), "tokq"], writes=[("DT", h)] + (QK4 if c == 0 else []))
                                S.op("dve", lambda e, bS=bS, h=h: e.tensor_tensor(out=scT4[:, h, :], in0=ps[bS][:, :], in1=DT4[:, h, :], op=ALU.mult),
                                     reads=[("ps", bS), ("DT", h)], writes=[("scT", h)])
                            PB = [0, 1, 2, 3]
                            UB = [4, 5, 6, 7]
                            DB = 4
                            den_ps = ps[DB][:, 300:308]
                            den8 = smal[:, 24:32].rearrange("p (h t) -> p h t", t=2)
                            d1 = smal[:, 32:36]
                            d2 = smal[:, 36:40]
                            rden4 = smal[:, 40:44]
                            ssn4 = smal[:, 44:48]
                            vv = smal[:, 48:52]
                            r4 = smal[:, 52:56]
                            for c in range(4):
                                cs = slice(c * 128, (c + 1) * 128)
                                wi4 = tokq[:, 16 + c * 4:16 + c * 4 + 4]
                                fl4 = tokq[:, 32 + c * 4:32 + c * 4 + 4]
                                for h in range(4):
                                    S.op("act", lambda e, c=c, h=h: e.activation(out=kw4[:, h, :], in_=ktok[:, c, h * 128:(h + 1) * 128], func=AF.Copy,
                                                                                 scale=tokq[:, 48 + c * 4 + h:48 + c * 4 + h + 1]),
                                         reads=[("ktok", h), "tokq"], writes=[("kw", h)])
                                for h in range(4):
                                    b = PB[h]
                                    S.op("pe", lambda e, b=b, h=h, cs=cs: e.matmul(ps[b][:, 0:256], lhsT=qkT[:, h, cs], rhs=Cb[:, h, 0:256], start=True, stop=True),
                                         reads=[("qkT", h), ("Cb", h)], writes=[("ps", b)], inc=False)
                                    S.op("pe", lambda e, b=b, h=h, cs=cs, c=c: e.matmul(ps[b][:, 256:512], lhsT=scT4[:, h, cs], rhs=vaug[:, c, h, 0:256], start=True, stop=True),
                                         reads=[("scT", h), ("vaug", c, h)], writes=[("ps", b)])
                                for h in range(4):
                                    S.op("pe", lambda e, h=h, cs=cs: e.matmul(ps[DB][:, 300 + 2 * h:301 + 2 * h], lhsT=qkT[:, h, cs], rhs=Cb[:, h, 256:257], start=True, stop=True),
                                         reads=[("qkT", h), ("Cb", h)], writes=[("ps", DB)], inc=False)
                                    S.op("pe", lambda e, h=h, cs=cs, c=c: e.matmul(ps[DB][:, 301 + 2 * h:302 + 2 * h], lhsT=scT4[:, h, cs], rhs=vaug[:, c, h, 256:257], start=True, stop=True),
                                         reads=[("scT", h), "vones"], writes=[("ps", DB)], inc=(h == 3))
                                S.op("act", lambda e: e.activation(out=smal[:, 24:32], in_=den_ps, func=AF.Copy), reads=[("ps", DB)], writes=[("smal", 24)])
                                for h in range(4):
                                    MM(UB[h], 0, 257, [(kw4[:, h, :], vaug[:, c, h, 0:257])], reads=[("kw", h), ("vaug", c, h), "vones"])
                                for h in range(4):
                                    b = PB[h]
                                    S.op("act", lambda e, b=b, h=h: e.activation(out=nd4[:, h, :], in_=ps[b][:, 256:512], func=AF.Copy), reads=[("ps", b)], writes=[("nd", h)])
                                    S.op("dve", lambda e, b=b, h=h, c=c: e.scalar_tensor_tensor(out=nd4[:, h, :], in0=ps[b][:, 0:256], scalar=tokq[:, 16 + c * 4 + h:16 + c * 4 + h + 1],
                                                                                             in1=nd4[:, h, :], op0=ALU.mult, op1=ALU.add),
                                         reads=[("ps", b), ("nd", h), "tokq"], writes=[("nd", h)])
                                for h in range(4):
                                    bU = UB[h]
                                    S.op("dve", lambda e, h=h, c=c, bU=bU: e.scalar_tensor_tensor(out=Cst[:, h, :], in0=Cst[:, h, :], scalar=wcb[:, h * 4 + c:h * 4 + c + 1],
                                                                                               in1=ps[bU][:, 0:257], op0=ALU.mult, op1=ALU.add),
                                         reads=[("Cst", h), "wcb", ("ps", bU)], writes=[("Cst", h)])
                                for h in range(4):
                                    S.op("act", lambda e, h=h: e.activation(out=tA[:, 0:256], in_=nd4[:, h, :], func=AF.Square, accum_out=ssn4[:, h:h + 1]),
                                         reads=[("nd", h)], writes=[("ssn", h)])
                                for h in range(4):
                                    S.op("act", lambda e, h=h: e.activation(out=Cb[:, h, 0:257], in_=Cst[:, h, :], func=AF.Copy), reads=[("Cst", h)], writes=[("Cb", h)])
                                S.op("dve", lambda e: e.tensor_tensor(out=d1, in0=den8[:, :, 0], in1=wi4, op=ALU.mult), reads=[("smal", 24), "tokq"], writes=[("smal", 32)])
                                S.op("dve", lambda e: e.tensor_tensor(out=d1, in0=d1, in1=den8[:, :, 1], op=ALU.add), reads=[("smal", 24), ("smal", 32)], writes=[("smal", 32)])
                                S.op("dve", lambda e: e.tensor_scalar(out=d2, in0=d1, scalar1=-1.0, scalar2=None, op0=ALU.mult), reads=[("smal", 32)], writes=[("smal", 36)])
                                S.op("dve", lambda e: e.tensor_tensor(out=d1, in0=d1, in1=d2, op=ALU.max), reads=[("smal", 32), ("smal", 36)], writes=[("smal", 32)])
                                S.op("dve", lambda e: e.tensor_tensor(out=d1, in0=d1, in1=fl4, op=ALU.max), reads=[("smal", 32), "tokq"], writes=[("smal", 32)])
                                S.op("dve", lambda e: e.reciprocal(out=rden4, in_=d1), reads=[("smal", 32)], writes=[("smal", 40)])
                                S.op("dve", lambda e: e.tensor_tensor(out=vv, in0=rden4, in1=rden4, op=ALU.mult), reads=[("smal", 40)], writes=[("smal", 48)])
                                S.op("dve", lambda e: e.tensor_tensor(out=vv, in0=vv, in1=ssn4, op=ALU.mult), reads=[("smal", 48)] + [("ssn", h) for h in range(4)], writes=[("smal", 48)])
                                S.op("dve", lambda e: e.tensor_scalar(out=vv, in0=vv, scalar1=1.0 / 256, scalar2=EPS, op0=ALU.mult, op1=ALU.add), reads=[("smal", 48)], writes=[("smal", 48)])
                                S.op("dve", lambda e: e.reciprocal(out=vv, in_=vv), reads=[("smal", 48)], writes=[("smal", 48)])
                                S.op("act", lambda e: e.activation(out=vv, in_=vv, func=AF.Sqrt), reads=[("smal", 48)], writes=[("smal", 48)])
                                S.op("dve", lambda e: e.tensor_tensor(out=r4, in0=vv, in1=rden4, op=ALU.mult), reads=[("smal", 48), ("smal", 40)], writes=[("smal", 52)])
                                for h in range(4):
                                    hs = slice(h * 256, (h + 1) * 256)
                                    S.op("dve", lambda e, c=c, h=h, hs=hs: e.scalar_tensor_tensor(out=sigo[:, c, hs], in0=nd4[:, h, :], scalar=smal[:, 52 + h:53 + h], in1=sigo[:, c, hs],
                                                                                               op0=ALU.mult, op1=ALU.mult),
                                         reads=[("nd", h), ("smal", 52), ("sigo", c, h)], writes=[("sigo", c, h)])
                            for c in range(4):
                                for half in range(2):
                                    b = nb()
                                    for q in range(4):
                                        ee = half * 4 + q
                                        S.op("pe", lambda e, b=b, q=q, ee=ee, c=c: e.transpose(out=psb[b][:, q * 128:(q + 1) * 128], in_=sigo[:, c, ee * 128:(ee + 1) * 128], identity=identb[:]),
                                             reads=[("sigo", c, ee // 2), "identb"], writes=[("ps", b)], inc=(q == 3))
                                    S.op("dve", lambda e, b=b, c=c, half=half: e.tensor_copy(out=hT[:, half * 4:half * 4 + 4, c * 128:(c + 1) * 128],
                                                                                          in_=psb[b][:, 0:512].rearrange("p (q t) -> p q t", q=4)),
                                         reads=[("ps", b)], writes=[("hT", half * 4 + q) for q in range(4)])
                            def cons_yo(mi, b):
                                S.op("act", lambda e, mi=mi, b=b: e.activation(out=ybuf[:, mi, :], in_=ps[b][:], func=AF.Copy),
                                     reads=[("ps", b)], writes=[("ybuf", mi)])
                                ysq(mi, b)
                            proj_fm(owout[j], 1024, 0, 1024, lambda k: hT[:, k, :], hTr, 8, cons_yo)
                        postnorm_add(1, l, pi, follow="sq")
                        S.barrier()
                        prenorm(2, l, pi, presq=True)
                        ar = Ar()
                        hid = ar.b16(22 * TP).rearrange("p (a t) -> p a t", a=22)
                        sg = [ar.f32(TP), ar.f32(TP)]
                        for j0 in range(0, 22, 2):
                            nj = 2
                            gv, gr = wload(wgu[l][:, j0 * 128:(j0 + nj) * 128].rearrange("(k p) n -> p k n", p=128), v3(8, nj * 128))
                            uv, ur = wload(wgu[l][:, 2816 + j0 * 128:2816 + (j0 + nj) * 128].rearrange("(k p) n -> p k n", p=128), v3(8, nj * 128))
                            hTk = [("hT", k) for k in range(8)]
                            bgs = [nb() for _ in range(4)]
                            if KFIRST and j0 == 0:
                                MMK(bgs, lambda i, k: (gv if i % 2 == 0 else uv)[:, k, (i // 2) * 128:(i // 2 + 1) * 128], lambda k: hT[:, k, :], 8,
                                    [gr, ur, gr, ur], hTk)
                            else:
                                for i, b in enumerate(bgs):
                                    wv_, wr_ = (gv, gr) if i % 2 == 0 else (uv, ur)
                                    MM(b, 0, TP, [(wv_[:, k, (i // 2) * 128:(i // 2 + 1) * 128], hT[:, k, :]) for k in range(8)],
                                       reads=[wr_], preads=[[hTk[k]] for k in range(8)])
                            for jj in range(nj):
                                jx = j0 + jj
                                bg, bu = bgs[2 * jj], bgs[2 * jj + 1]
                                sgt = sg[jx % 2]
                                S.op("act", lambda e, bg=bg, sgt=sgt: e.activation(out=sgt, in_=ps[bg][:], func=AF.Silu),
                                     reads=[("ps", bg)], writes=[("sg", jx % 2)])
                                S.op("dve", lambda e, bu=bu, sgt=sgt, jx=jx: e.tensor_tensor(out=hid[:, jx, :], in0=ps[bu][:], in1=sgt, op=ALU.mult),
                                     reads=[("ps", bu), ("sg", jx % 2)], writes=[("hid", jx)])
                        for m in range(8):
                            dvs = []
                            for jh in range(2):
                                dv, dr = wload(wdn[l][jh * 1408:(jh + 1) * 1408, m * 128:(m + 1) * 128].rearrange("(j p) n -> p j n", p=128), v3(11, 128))
                                dvs.append((dv, dr))
                            b = nb()
                            MM(b, 0, TP, [(dvs[jx // 11][0][:, jx % 11, :], hid[:, jx, :]) for jx in range(22)], reads=[],
                               preads=[[dvs[jx // 11][1], ("hid", jx)] for jx in range(22)])
                            S.op("act", lambda e, m=m, b=b: e.activation(out=ybuf[:, m, :], in_=ps[b][:], func=AF.Copy),
                                 reads=[("ps", b)], writes=[("ybuf", m)])
                            ysq(m, b)
                        postnorm_add(3, l, pi, follow="cast")
                        ar = Ar()
                        ar.off = 6656
                        sgp = [ar.f32(TP), ar.f32(TP)]
                        for kk in range(2):
                            b = nb()
                            for c in range(4):
                                S.op("pe", lambda e, b=b, c=c, kk=kk: e.transpose(out=psb[b][:, c * 128:(c + 1) * 128], in_=pin[:, c, kk * 128:(kk + 1) * 128],
                                                                                 identity=identb[:]),
                                     reads=["pin", "identb"], writes=[("ps", b)], inc=(c == 3))
                            S.op("dve", lambda e, b=b, kk=kk: e.tensor_copy(out=pT[:, kk, :], in_=psb[b][:, 0:512]), reads=[("ps", b)], writes=[("pT", kk)])
                        prv, prr = wload(pproj[l].rearrange("(k p) n -> p k n", p=128), v3(2, 1024))
                        for mi in range(8):
                            b2 = nb()
                            MM(b2, 0, TP, [(prv[:, k, mi * 128:(mi + 1) * 128], pT[:, k, :]) for k in range(2)],
                               reads=[prr, ("pT", 0), ("pT", 1)])
                            S.op("act", lambda e, b2=b2, mi=mi: e.activation(out=ybuf[:, mi, :], in_=ps[b2][:], func=AF.Copy),
                                 reads=[("ps", b2)], writes=[("ybuf", mi)])

                        def cons_e(mi, b):
                            sgt = sgp[mi % 2]
                            S.op("act", lambda e, b=b, sgt=sgt: e.activation(out=sgt, in_=ps[b][:], func=AF.Sigmoid),
                                 reads=[("ps", b)], writes=[("sgp", mi % 2)])
                            S.op("dve", lambda e, sgt=sgt, mi=mi: e.tensor_tensor(out=ybuf[:, mi, :], in0=ybuf[:, mi, :], in1=sgt, op=ALU.mult),
                                 reads=[("ybuf", mi), ("sgp", mi % 2)], writes=[("ybuf", mi)])
                            S.op("dve", lambda e, mi=mi: e.tensor_tensor(out=sq[:, mi, :], in0=ybuf[:, mi, :], in1=ybuf[:, mi, :], op=ALU.mult),
                                 reads=[("ybuf", mi)], writes=[("sq", mi)])
                        proj_fm(pgate[l], 1024, 0, 1024, lambda k: hT[:, k, :], [("hT", k) for k in range(8)], 8, cons_e, kfirst=True)
                        postnorm_add(4, l, pi)

                    for pi in range(NPASS):
                        do_pass(s, l, j, even, pi)

                S.barrier()
                ar = Ar()
                xo = [ar.f32(1024), ar.f32(1024)]
                def store_tile(s, tt):
                    xi = xo[tt % 2]
                    pi_ = (tt * 128) // TP
                    for half in range(2):
                        b = nb()
                        for q in range(4):
                            c = half * 4 + q
                            S.op("pe", lambda e, c=c, q=q, b=b: e.transpose(out=ps[b][:, q * 128:(q + 1) * 128],
                                                                          in_=xT[:, c, tt * 128:(tt + 1) * 128], identity=ident),
                                 reads=[("x", c, pi_), "cst"], writes=[("ps", b)], inc=(q == 3))
                        if half == 0:
                            S.op("dve", lambda e, b=b, xi=xi: e.tensor_copy(out=xi[:, 0:512], in_=ps[b][:]), reads=[("ps", b)],
                                 writes=[("xo", tt % 2, 0)])
                        else:
                            S.op("act", lambda e, b=b, xi=xi: e.activation(out=xi[:, 512:1024], in_=ps[b][:], func=AF.Copy), reads=[("ps", b)],
                                 writes=[("xo", tt % 2, 1)])
                    out_evs.append(S.dma("sp", out_d[s, tt * 128:(tt + 1) * 128, :], xi, reads=[("xo", tt % 2, 0), ("xo", tt % 2, 1)]))

                for tt in range(NTT):
                    store_tile(s, tt)

            S.finish("sp", out_evs)
            return wlist

        S0 = Sched(nc, sems)
        plan = record(S0, None)
        S = Sched(nc, sems)
        record(S, plan)
        S.emit()
    return nc


def _consts():
    c = np.zeros((128, NCONST), np.float32)
    c[:, 0:128] = np.eye(128, dtype=np.float32)
    s = np.arange(128)[:, None]
    t = np.arange(128)[None, :]
    tri = (s <= t).astype(np.float32)
    c[:, 128:256] = tri
    c[:, 256:768] = np.tile(np.where(s <= t, 0.0, NEGBIG).astype(np.float32), (1, 4))
    for h in range(4):
        c[h, 768 + h * 128:768 + (h + 1) * 128] = 1.0
    for g, win in enumerate((2, 4, 8, 16)):
        c[:, 1280 + g * 16:1280 + (g + 1) * 16] = (1.0 / np.minimum(np.arange(16) + 1, win)).astype(np.float32)[None, :]
    c[:, 1344:1472] = 1.0
    return c


def _prep_shared(inp):
    f = lambda a: np.ascontiguousarray(np.asarray(a, dtype=np.float32))
    vecs = np.zeros((128, NV), np.float32)
    kinds = ["mix_pre_gain", "mix_post_gain", "ffn_pre_gain", "ffn_post_gain", "ple_post_gain"]
    for k, name in enumerate(kinds):
        g = f(inp[name])
        vecs[:, k * 32:(k + 1) * 32] = g.reshape(4, 8, 128).transpose(2, 0, 1).reshape(128, 32)
    vecs[:, 160:168] = f(inp["even_b_scale"]).reshape(2, 4, 128).transpose(2, 0, 1).reshape(128, 8)
    vecs[:, 168:232] = f(inp["odd_conv_w"]).reshape(2, 4, 8, 128).transpose(3, 0, 1, 2).reshape(128, 64)
    vecs[0:4, 232:234] = f(inp["odd_b_i"]).T
    vecs[0:4, 234:236] = f(inp["odd_b_f"]).T
    bce = np.zeros((2, 128, 2560), np.float32)
    bce[:, :, 0:512] = f(inp["even_a_v_gain"])[:, None, :]
    bs = f(inp["even_a_bs"])
    bce[:, :, 512:2560] = np.tile(bs[:, :, None, :], (1, 1, 4, 1)).reshape(2, 1, 2048)
    bco = np.ascontiguousarray(np.broadcast_to(f(inp["odd_h_gain"])[:, None, :], (2, 128, 1024)))
    d = {
        "even_w_in": f(inp["even_w_in"]), "even_w_out": f(inp["even_w_out"]), "even_b_wpool": f(inp["even_b_wpool"]),
        "ws_t": np.ascontiguousarray(f(inp["even_a_ws"]).transpose(0, 1, 3, 2)),
        "odd_w_in": f(inp["odd_w_in"]), "odd_w_out": f(inp["odd_w_out"]),
        "ffn_w_gate_up": f(inp["ffn_w_gate_up"]), "ffn_w_down": f(inp["ffn_w_down"]),
        "ple_proj": f(inp["ple_proj"]), "ple_gate": f(inp["ple_gate"]),
        "vecs": vecs, "bc_even": bce, "bc_odd": bco, "consts": _consts(),
    }
    return d


def run(inp, n_cores, NSEQ, T, NL, trace=False):
    x = np.asarray(inp["x"], dtype=np.float32)
    p = np.asarray(inp["p"], dtype=np.float32)
    shared = _prep_shared(inp)
    nc = build_nc(NSEQ, T, NL)
    in_maps = []
    for c in range(n_cores):
        m = dict(shared)
        m["x"] = np.ascontiguousarray(x[c * NSEQ:(c + 1) * NSEQ])
        m["p"] = np.ascontiguousarray(p[:, c * NSEQ:(c + 1) * NSEQ])
        in_maps.append(m)
    res = run_bass_kernel_spmd(nc, in_maps, core_ids=list(range(n_cores)), trace=trace)
    out = np.concatenate([r["out"] for r in res.results], axis=0)
    return out, res


def kernel(**inputs):
    out, _ = run(inputs, 8, 2, 2048, 4)
    return out.astype(np.float32)
```

```python
import numpy as np
from contextlib import ExitStack
import concourse.bass as bass
import concourse.mybir as mybir
from concourse.bass_utils import run_bass_kernel_spmd

F32 = mybir.dt.float32
BF16 = mybir.dt.bfloat16
ALU = mybir.AluOpType
AF = mybir.ActivationFunctionType

ENGS = ("pe", "act", "dve", "pool", "sp")
NDMASEM = 8
EPS = 1e-6
NV = 236
NCONST = 1472
TP = 512
NSLOT = 6
WLOOK = 3
WSZ = 2048
NEGBIG = -30000.0
KFIRST = False


class Sched:
    def __init__(self, nc, sems):
        self.nc = nc
        self.ops = {e: [] for e in ENGS}
        self.sem, self.dsem = sems
        self.cnt = {e: 0 for e in ENGS}
        self.dcnt = {q: 0 for q in ("sp", "pool")}
        self.seen = {}
        self.last_w = {}
        self.readers = {}

    def _semh(self, k):
        return self.sem[k] if isinstance(k, str) else self.dsem[k[0]][k[1]]

    def _need(self, eng, ev, waits):
        if ev is None:
            return
        k, v = ev
        if k == "pe" and eng == "pe":
            return
        if self.seen.get((eng, k), 0) >= v:
            return
        if isinstance(k, str):
            assert v <= self.cnt[k], ("wait on un-incremented event", eng, k, v, self.cnt[k])
        self.seen[(eng, k)] = v
        waits.append((k, v))

    def _deps(self, eng, reads, writes):
        waits = []
        for r in reads:
            self._need(eng, self.last_w.get(r), waits)
        for w in writes:
            self._need(eng, self.last_w.get(w), waits)
            for ev in self.readers.get(w, ()):
                self._need(eng, ev, waits)
        return waits

    def _commit(self, ev, reads, writes):
        for r in reads:
            self.readers.setdefault(r, []).append(ev)
        for w in writes:
            self.last_w[w] = ev
            self.readers[w] = []

    def op(self, eng, fn, reads=(), writes=(), inc=True):
        waits = self._deps(eng, reads, writes)
        ev = (eng, self.cnt[eng] + 1)
        if inc:
            self.cnt[eng] += 1
        self.ops[eng].append((waits, fn, (eng, 1) if inc else None))
        self._commit(ev, reads, writes)
        return ev

    def dma(self, q, out, in_, reads=(), writes=()):
        i = self.dcnt[q]
        self.dcnt[q] += 1
        slot = i % NDMASEM
        k = (q, slot)
        waits = self._deps(q, reads, writes)
        prev = i // NDMASEM
        if prev > 0:
            self._need(q, (k, 16 * prev), waits)
        ev = (k, 16 * (prev + 1))
        self.ops[q].append((waits, lambda e: e.dma_start(out=out, in_=in_), (k, 16)))
        self._commit(ev, reads, writes)
        return ev

    def barrier(self, engs=("pe", "act", "dve", "pool")):
        for e in engs:
            waits = []
            for k in engs:
                if k != e and self.cnt[k] > 0:
                    self._need(e, (k, self.cnt[k]), waits)
            if waits:
                self.ops[e].append((waits, None, None))

    def finish(self, eng, evs):
        waits = []
        for ev in evs:
            self._need(eng, ev, waits)
        self.ops[eng].append((waits, None, None))

    def emit(self):
        nc = self.nc
        engmap = {"pe": "tensor", "act": "scalar", "dve": "vector", "pool": "gpsimd", "sp": "sync"}
        with nc.Block() as block:
            for e in ENGS:
                ops = self.ops[e]

                def body(engine, ops=ops):
                    for waits, fn, inc in ops:
                        for k, v in waits:
                            engine.wait_ge(self._semh(k), v)
                        if fn is None:
                            continue
                        ins = fn(engine)
                        if inc is not None:
                            ins.then_inc(self._semh(inc[0]), inc[1])

                getattr(block, engmap[e])(body)


def build_nc(NSEQ=2, T=2048, NL=4):
    nc = bass.Bass("TRN2", target_bir_lowering=False)
    D = 1024
    NPASS = T // TP
    NTT = T // 128

    def din(name, shape):
        return nc.dram_tensor(name, list(shape), F32, kind="ExternalInput").ap()

    x_d = din("x", [NSEQ, T, D])
    p_d = din("p", [4, NSEQ, T, 256])
    ewin = din("even_w_in", [2, 1024, 1536])
    ewout = din("even_w_out", [2, 1024, 1024])
    ewpool = din("even_b_wpool", [2, 4, 128, 128])
    wst_d = din("ws_t", [2, 4, 128, 128])
    owin = din("odd_w_in", [2, 1024, 3080])
    owout = din("odd_w_out", [2, 1024, 1024])
    wgu = din("ffn_w_gate_up", [4, 1024, 5632])
    wdn = din("ffn_w_down", [4, 2816, 1024])
    pproj = din("ple_proj", [4, 256, 1024])
    pgate = din("ple_gate", [4, 1024, 1024])
    vecs_d = din("vecs", [128, NV])
    bce_d = din("bc_even", [2, 128, 2560])
    bco_d = din("bc_odd", [2, 128, 1024])
    consts_d = din("consts", [128, NCONST])
    out_d = nc.dram_tensor("out", [NSEQ, T, D], F32, kind="ExternalOutput").ap()

    with ExitStack() as st:
        sems = ({e: st.enter_context(nc.semaphore("s_" + e)) for e in ENGS},
                {q: [st.enter_context(nc.semaphore("d_%s%d" % (q, i))) for i in range(NDMASEM)] for q in ("sp", "pool")})

        def sb(n, sh, dt):
            return st.enter_context(nc.sbuf_tensor("sb_" + n, list(sh), dt))

        xT = sb("xT", [128, 8, T], F32)
        cst = sb("cst", [128, NCONST], F32)
        vecs = sb("vecs", [128, NV], F32)
        nbf = sb("nbf", [128, 2], F32)
        lbc = sb("lbc", [128, 2560], F32)
        identb = sb("identb", [128, 128], BF16)
        onesb = sb("onesb", [128, 128], BF16)
        hT = sb("hT", [128, 8, TP], BF16)
        ybuf = sb("ybuf", [128, 8, TP], F32)
        sq = sb("sq", [128, 8, TP], BF16)
        rstd = sb("rstd", [128, TP], F32)
        tA = sb("tA", [128, TP], F32)
        tB = [sb("tB%d" % i, [128, TP], F32) for i in range(2)]
        wsl = [sb("wsl%d" % i, [128, WSZ], BF16) for i in range(NSLOT)]
        pin = sb("pin", [128, 4, 256], BF16)
        pT = sb("pT", [128, 2, TP], BF16)
        WT = sb("WT", [128, 4, 128], BF16)
        wstf = sb("wstf", [128, 4, 128], F32)
        halo_e = sb("halo_e", [128, 4, 16], F32)
        halo_o = sb("halo_o", [128, 8, 4], F32)
        Cst = sb("Cst", [128, 4, 257], F32)
        Cb = sb("Cb", [128, 4, 258], BF16)
        mprev = sb("mprev", [4, 8], F32)
        smal = sb("smal", [128, 64], F32)
        NA = 12288
        arena = sb("arena", [128, NA], F32)
        ps = [st.enter_context(nc.psum_tensor("ps%d" % i, [128, 512], F32)) for i in range(8)]
        psb = [p_[:].bitcast(BF16) for p_ in ps]
        def record(S, plan):
            bank = [0]

            def nb():
                b = bank[0]
                bank[0] = (b + 1) % 8
                return b

            class Ar:
                def __init__(self):
                    self.off = 0

                def f32(self, n):
                    a = arena[:, self.off:self.off + n]
                    self.off += n
                    assert self.off <= NA, self.off
                    return a

                def b16(self, n):
                    w = (n + 1) // 2
                    a = arena[:, self.off:self.off + w].bitcast(BF16)
                    self.off += w
                    assert self.off <= NA, self.off
                    return a

            ident = cst[:, 0:128]
            triu = cst[:, 128:256]
            maskneg4 = cst[:, 256:768]
            invcnt = cst[:, 1280:1344]

            def gcol(kind, l, c):
                return vecs[:, (kind * 4 + l) * 8 + c:(kind * 4 + l) * 8 + c + 1]

            def MM(b, lo, hi, pairs, reads, rows=128, preads=None):
                n = len(pairs)
                for i, (l, r) in enumerate(pairs):
                    rd = list(reads) if i == 0 else []
                    if preads is not None:
                        rd += list(preads[i])
                    S.op("pe", lambda e, l=l, r=r, i=i: e.matmul(ps[b][0:rows, lo:hi], lhsT=l, rhs=r,
                                                                  start=(i == 0), stop=(i == n - 1)),
                         reads=rd, writes=[("ps", b)], inc=(i == n - 1))

            wcount = [0]
            wissued = [0]
            wlist = []

            def wload(src_ap, view):
                i = wcount[0]
                wcount[0] += 1
                wlist.append((src_ap, view))
                if plan is None:
                    S.dma("pool", view(wsl[i % NSLOT]), src_ap, writes=[("w", i % NSLOT)])
                else:
                    while wissued[0] < min(i + WLOOK + 1, len(plan)):
                        n = wissued[0]
                        wissued[0] += 1
                        src_n, view_n = plan[n]
                        S.dma("pool", view_n(wsl[n % NSLOT]), src_n, writes=[("w", n % NSLOT)])
                return view(wsl[i % NSLOT]), ("w", i % NSLOT)

            def v3(k, n):
                return lambda s_: s_[:, 0:k * n].rearrange("p (k n) -> p k n", k=k)

            def norm_rstd(srcs, regs, Dn, presq=False):
                nch = len(srcs)
                if not presq:
                    for c in range(nch):
                        if c % 2 == 0:
                            S.op("act", lambda e, c=c: e.activation(out=sq[:, c, :], in_=srcs[c], func=AF.Square),
                                 reads=[regs[c]], writes=[("sq", c)])
                        else:
                            S.op("dve", lambda e, c=c: e.tensor_tensor(out=sq[:, c, :], in0=srcs[c], in1=srcs[c], op=ALU.mult),
                                 reads=[regs[c]], writes=[("sq", c)])
                b = nb()
                MM(b, 0, TP, [(onesb[:], sq[:, c, :]) for c in range(nch)], reads=["onesb"],
                   preads=[[("sq", c)] for c in range(nch)])
                S.op("act", lambda e: e.activation(out=tA[:], in_=ps[b][:], func=AF.Ln, scale=1.0 / Dn, bias=EPS),
                     reads=[("ps", b)], writes=["tA"])
                S.op("act", lambda e: e.activation(out=rstd[:], in_=tA[:], func=AF.Exp, scale=-0.5), reads=["tA"], writes=["rstd"])

            def xs(c, pi):
                return xT[:, c, pi * TP:(pi + 1) * TP]

            def prenorm(kind, l, pi, presq=False):
                norm_rstd([xs(c, pi) for c in range(8)], [("x", c, pi) for c in range(8)], D, presq=presq)
                for c in range(8):
                    S.op("dve", lambda e, c=c: e.scalar_tensor_tensor(out=hT[:, c, :], in0=xs(c, pi), scalar=gcol(kind, l, c),
                                                                      in1=rstd[:], op0=ALU.mult, op1=ALU.mult),
                         reads=[("x", c, pi), "rstd", "vecs"], writes=[("hT", c)])

            def ysq(mi, b):
                S.op("dve", lambda e, mi=mi, b=b: e.tensor_tensor(out=sq[:, mi, :], in0=ps[b][:], in1=ybuf[:, mi, :], op=ALU.mult),
                     reads=[("ps", b), ("ybuf", mi)], writes=[("sq", mi)])

            def postnorm_add(kind, l, pi, follow=None):
                norm_rstd([ybuf[:, m, :] for m in range(8)], [("ybuf", m) for m in range(8)], D, presq=True)
                for m in range(8):
                    t = tB[m % 2]
                    S.op("dve", lambda e, m=m, t=t: e.scalar_tensor_tensor(out=t[:], in0=ybuf[:, m, :], scalar=gcol(kind, l, m),
                                                                           in1=rstd[:], op0=ALU.mult, op1=ALU.mult),
                         reads=[("ybuf", m), "rstd", "vecs"], writes=[("tB", m % 2)])
                    S.op("dve", lambda e, m=m, t=t: e.tensor_tensor(out=xs(m, pi), in0=xs(m, pi), in1=t[:], op=ALU.add),
                         reads=[("tB", m % 2), ("x", m, pi)], writes=[("x", m, pi)])
                    if follow == "sq":
                        S.op("act", lambda e, m=m: e.activation(out=sq[:, m, :], in_=xs(m, pi), func=AF.Square),
                             reads=[("x", m, pi)], writes=[("sq", m)])
                    elif follow == "cast":
                        S.op("act", lambda e, m=m: e.activation(out=hT[:, m, :], in_=xs(m, pi), func=AF.Copy),
                             reads=[("x", m, pi)], writes=[("hT", m)])

            def MMK(banks, lhs, rhs_fn, nk, wreads, rhs_reads):
                for k in range(nk):
                    for i, b in enumerate(banks):
                        rd = [rhs_reads[k]] if i == 0 else []
                        if k == 0:
                            rd = rd + [wreads[i]]
                        S.op("pe", lambda e, i=i, b=b, k=k: e.matmul(ps[b][:, 0:TP], lhsT=lhs(i, k), rhs=rhs_fn(k), start=(k == 0), stop=(k == nk - 1)),
                             reads=rd, writes=[("ps", b)], inc=(k == nk - 1))

            def proj_fm(wsrc, ncols_total, col0, nblk_cols, rhs_fn, rhs_reads, nk, consume, kfirst=False):
                nblocks = (nblk_cols + 255) // 256
                mi = 0
                b0 = 0
                if KFIRST and kfirst and nblocks >= 2 and nblk_cols >= 512:
                    wvs = []
                    for bi in range(2):
                        c0 = col0 + bi * 256
                        wvs.append(wload(wsrc[:, c0:c0 + 256].rearrange("(k p) n -> p k n", p=128), v3(nk, 256)))
                    banks = [nb() for _ in range(4)]
                    MMK(banks, lambda i, k: wvs[i // 2][0][:, k, (i % 2) * 128:(i % 2 + 1) * 128], rhs_fn, nk,
                        [wvs[i // 2][1] for i in range(4)], rhs_reads)
                    for b in banks:
                        consume(mi, b)
                        mi += 1
                    b0 = 2
                for bi in range(b0, nblocks):
                    c0 = col0 + bi * 256
                    w = min(256, nblk_cols - bi * 256)
                    wv, wr = wload(wsrc[:, c0:c0 + w].rearrange("(k p) n -> p k n", p=128), v3(nk, w))
                    for mm in range(w // 128):
                        b = nb()
                        MM(b, 0, TP, [(wv[:, k, mm * 128:(mm + 1) * 128], rhs_fn(k)) for k in range(nk)],
                           reads=[wr], preads=[[rhs_reads[k]] for k in range(nk)])
                        consume(mi, b)
                        mi += 1

            S.dma("sp", cst[:], consts_d, writes=["cst"])
            S.dma("sp", vecs[:], vecs_d, writes=["vecs"])
            S.op("act", lambda e: e.activation(out=identb[:], in_=ident, func=AF.Copy), reads=["cst"], writes=["identb"])
            S.op("dve", lambda e: e.memset(onesb[:], 1.0), writes=["onesb"])
            S.op("dve", lambda e: e.tensor_scalar(out=nbf[0:4, :], in0=vecs[0:4, 234:236], scalar1=-1.0, scalar2=None, op0=ALU.mult),
                 reads=["vecs"], writes=["nbf"])
            out_evs = []

            for s in range(NSEQ):
                S.barrier()
                ar = Ar()
                xin = [ar.f32(1024), ar.f32(1024)]
                def load_tile(s, tt):
                    xi = xin[tt % 2]
                    S.dma("sp", xi, x_d[s, tt * 128:(tt + 1) * 128, :], writes=[("xo", tt % 2, 0), ("xo", tt % 2, 1)])
                    for half in range(2):
                        b = nb()
                        for q in range(4):
                            c = half * 4 + q
                            S.op("pe", lambda e, c=c, q=q, b=b, xi=xi: e.transpose(out=ps[b][:, q * 128:(q + 1) * 128],
                                                                                  in_=xi[:, c * 128:(c + 1) * 128], identity=ident),
                                 reads=[("xo", tt % 2, 0), ("xo", tt % 2, 1), "cst"], writes=[("ps", b)], inc=(q == 3))
                        eng = "dve" if half == 0 else "act"
                        dst = xT[:, half * 4:half * 4 + 4, tt * 128:(tt + 1) * 128]
                        src = ps[b][:, :].rearrange("p (q t) -> p q t", q=4)
                        pi_ = (tt * 128) // TP
                        if eng == "dve":
                            S.op("dve", lambda e, dst=dst, src=src: e.tensor_copy(out=dst, in_=src), reads=[("ps", b)],
                                 writes=[("x", half * 4 + q, pi_) for q in range(4)])
                        else:
                            S.op("act", lambda e, dst=dst, src=src: e.activation(out=dst, in_=src, func=AF.Copy), reads=[("ps", b)],
                                 writes=[("x", half * 4 + q, pi_) for q in range(4)])

                for tt in range(NTT):
                    load_tile(s, tt)

                for l in range(NL):
                    j = l // 2
                    even = (l % 2 == 0)
                    S.barrier()
                    if even:
                        S.dma("sp", lbc[:, 0:2560], bce_d[j], writes=["lbc"])
                        S.dma("sp", wstf[:], wst_d[j].rearrange("h s t -> s h t"), writes=["wstf"])
                        for h in range(4):
                            S.op("dve", lambda e, h=h: e.tensor_tensor(out=WT[:, h, :], in0=wstf[:, h, :], in1=triu, op=ALU.mult),
                                 reads=["wstf", "cst"], writes=["WT"])
                    else:
                        S.dma("sp", lbc[:, 0:1024], bco_d[j], writes=["lbc"])
                    def do_pass(s, l, j, even, pi):
                        cols = slice(pi * TP, (pi + 1) * TP)
                        S.dma("pool", pin[:], p_d[l, s, cols, :].rearrange("(c p) f -> p c f", p=128), writes=["pin"])
                        S.barrier()
                        prenorm(0, l, pi)
                        ar = Ar()
                        if even:
                            u = ar.b16(4 * TP).rearrange("p (a t) -> p a t", a=4)
                            vg = [ar.f32(TP) for _ in range(4)]
                            vn = ar.b16(4 * 512).rearrange("p (c f) -> p c f", c=4)
                            xbuf = ar.f32(4 * 528).rearrange("p (g t) -> p g t", g=4)
                            ptmp = [ar.f32(528), ar.f32(528)]
                            pooled = ar.b16(4 * TP).rearrange("p (g t) -> p g t", g=4)
                            ycat = ar.b16(8 * TP).rearrange("p (a t) -> p a t", a=8)
                            wsrc = ewin[j]
                            def cons_u(mi, b):
                                S.op("act", lambda e, mi=mi, b=b: e.activation(out=u[:, mi, :], in_=ps[b][:], func=AF.Gelu_apprx_tanh),
                                     reads=[("ps", b)], writes=[("u", mi)])
                            proj_fm(wsrc, 1536, 0, 512, lambda k: hT[:, k, :], [("hT", k) for k in range(8)], 8, cons_u, kfirst=True)
                            if pi == 0:
                                S.op("dve", lambda e: e.memset(xbuf[:, :, 0:16], 0.0), writes=[("xbuf", g) for g in range(4)])
                            else:
                                S.op("dve", lambda e: e.tensor_copy(out=xbuf[:, :, 0:16], in_=halo_e[:]), reads=["halo_e"],
                                     writes=[("xbuf", g) for g in range(4)])

                            def cons_xb(mi, b):
                                S.op("act", lambda e, mi=mi, b=b: e.activation(out=xbuf[:, mi, 16:528], in_=ps[b][:], func=AF.Copy),
                                     reads=[("ps", b)], writes=[("xbuf", mi)])
                            proj_fm(wsrc, 1536, 1024, 512, lambda k: hT[:, k, :], [("hT", k) for k in range(8)], 8, cons_xb)
                            S.op("dve", lambda e: e.tensor_copy(out=halo_e[:], in_=xbuf[:, :, 512:528]), reads=[("xbuf", g) for g in range(4)],
                                 writes=["halo_e"])
                            vb = [nb() for _ in range(4)]
                            for bi in range(2):
                                wv, wr = wload(wsrc[:, 512 + bi * 256:512 + (bi + 1) * 256].rearrange("(k p) n -> p k n", p=128), v3(8, 256))
                                for tk in range(4):
                                    MM(vb[tk], bi * 256, (bi + 1) * 256, [(hT[:, k, tk * 128:(tk + 1) * 128], wv[:, k, :]) for k in range(8)],
                                       reads=[wr], preads=[[("hT", k)] for k in range(8)])
                            wpv, wpr = wload(ewpool[j].rearrange("g d e -> d g e"), v3(4, 128))
                            for g in range(4):
                                win = 2 << g
                                src = xbuf[:, g, :]
                                lo = 0
                                for lev in range(g + 1):
                                    sh = 1 << lev
                                    dstt = ptmp[lev % 2]
                                    nlo = lo + sh
                                    S.op("dve", lambda e, src=src, dstt=dstt, nlo=nlo, sh=sh: e.tensor_tensor(
                                        out=dstt[:, nlo:528], in0=src[:, nlo:528], in1=src[:, nlo - sh:528 - sh], op=ALU.add),
                                        reads=[("xbuf", g), ("ptmp", (lev + 1) % 2)], writes=[("ptmp", lev % 2)])
                                    src = dstt
                                    lo = nlo
                                fin = src
                                S.op("dve", lambda e, fin=fin, g=g, win=win: e.scalar_tensor_tensor(
                                    out=pooled[:, g, :], in0=fin[:, 16:528], scalar=1.0 / win, in1=xbuf[:, g, 16:528],
                                    op0=ALU.mult, op1=ALU.subtract),
                                    reads=[("ptmp", g % 2), ("xbuf", g)], writes=[("pooled", g)])
                                if pi == 0:
                                    S.op("dve", lambda e, fin=fin, g=g: e.tensor_tensor(out=tA[:, 0:16], in0=fin[:, 16:32], in1=invcnt[:, g * 16:(g + 1) * 16], op=ALU.mult),
                                         reads=[("ptmp", g % 2), "cst"], writes=["tA"])
                                    S.op("dve", lambda e, g=g: e.tensor_tensor(out=pooled[:, g, 0:16], in0=tA[:, 0:16], in1=xbuf[:, g, 16:32], op=ALU.subtract),
                                         reads=["tA", ("xbuf", g)], writes=[("pooled", g)])
                                b = nb()
                                MM(b, 0, TP, [(wpv[:, g, :], pooled[:, g, :])], reads=[wpr, ("pooled", g)])
                                S.op("act", lambda e, b=b, g=g: e.activation(out=ycat[:, 4 + g, :], in_=ps[b][:], func=AF.Copy,
                                                                             scale=vecs[:, 160 + j * 4 + g:160 + j * 4 + g + 1]),
                                     reads=[("ps", b), "vecs"], writes=[("ycat", 4 + g)])
                            for tk in range(4):
                                b = vb[tk]
                                S.op("act", lambda e, b=b, tk=tk: e.activation(out=vg[tk], in_=ps[b][:], func=AF.Gelu_apprx_tanh),
                                     reads=[("ps", b)], writes=[("vg", tk)])
                            for tk in range(4):
                                S.op("act", lambda e, tk=tk: e.activation(out=tA[:], in_=vg[tk], func=AF.Square, accum_out=smal[:, tk:tk + 1]),
                                     reads=[("vg", tk)], writes=[("smal", tk)])
                            S.op("dve", lambda e: e.tensor_scalar(out=smal[:, 0:4], in0=smal[:, 0:4], scalar1=1.0 / 512, scalar2=EPS, op0=ALU.mult, op1=ALU.add),
                                 reads=[("smal", tk) for tk in range(4)], writes=[("smal", tk) for tk in range(4)])
                            S.op("dve", lambda e: e.reciprocal(out=smal[:, 0:4], in_=smal[:, 0:4]),
                                 reads=[("smal", tk) for tk in range(4)], writes=[("smal", tk) for tk in range(4)])
                            S.op("act", lambda e: e.activation(out=smal[:, 0:4], in_=smal[:, 0:4], func=AF.Sqrt),
                                 reads=[("smal", tk) for tk in range(4)], writes=[("smal", tk) for tk in range(4)])
                            for tk in range(4):
                                S.op("dve", lambda e, tk=tk: e.scalar_tensor_tensor(out=vn[:, tk, :], in0=vg[tk], scalar=smal[:, tk:tk + 1],
                                                                                    in1=lbc[:, 0:512], op0=ALU.mult, op1=ALU.mult),
                                     reads=[("vg", tk), ("smal", tk), "lbc"], writes=[("vn", tk)])
                            for h in range(4):
                                b = nb()
                                for tk in range(4):
                                    S.op("pe", lambda e, b=b, tk=tk, h=h: e.matmul(ps[b][:, tk * 128:(tk + 1) * 128], lhsT=vn[:, tk, h * 128:(h + 1) * 128],
                                                                                  rhs=WT[:, h, :], start=True, stop=True),
                                         reads=[("vn", tk), "WT"], writes=[("ps", b)], inc=(tk == 3))
                                t = tB[h % 2]
                                S.op("dve", lambda e, b=b, h=h, t=t: e.tensor_tensor(out=t[:], in0=ps[b][:], in1=lbc[:, 512 + h * 512:512 + (h + 1) * 512], op=ALU.add),
                                     reads=[("ps", b), "lbc"], writes=[("tB", h % 2)])
                                S.op("dve", lambda e, h=h, t=t: e.tensor_tensor(out=ycat[:, h, :], in0=t[:], in1=u[:, h, :], op=ALU.mult),
                                     reads=[("tB", h % 2), ("u", h)], writes=[("ycat", h)])
                            def cons_y(mi, b):
                                S.op("act", lambda e, mi=mi, b=b: e.activation(out=ybuf[:, mi, :], in_=ps[b][:], func=AF.Copy),
                                     reads=[("ps", b)], writes=[("ybuf", mi)])
                                ysq(mi, b)
                            proj_fm(ewout[j], 1024, 0, 1024, lambda k: ycat[:, k, :], [("ycat", k) for k in range(8)], 8, cons_y)
                        else:

                            ones4 = cst[0:4, 1344:1472]
                            qk_raw = ar.f32(4 * 516)
                            qkpre = qk_raw.rearrange("p (a t) -> p a t", a=4)
                            DT4 = qk_raw[:, 0:2048].rearrange("p (h t) -> p h t", h=4)
                            qkT = ar.b16(8 * TP).rearrange("p (a t) -> p a t", a=8)
                            ktok = ar.b16(4 * 512).rearrange("p (c f) -> p c f", c=4)
                            vaug = ar.b16(16 * 258).rearrange("p (c h f) -> p c h f", c=4, h=4)
                            sigo = ar.b16(4 * 1024).rearrange("p (c f) -> p c f", c=4)
                            scT4 = ar.b16(4 * 512).rearrange("p (h t) -> p h t", h=4)
                            nd4 = ar.f32(4 * 256).rearrange("p (h f) -> p h f", h=4)
                            kw4 = ar.b16(4 * 128).rearrange("p (h f) -> p h f", h=4)
                            tokq = ar.f32(64)
                            wcb = ar.f32(16)
                            wsrc = owin[j]
                            hTr = [("hT", k) for k in range(8)]

                            def cw(tap, m):
                                cidx = 168 + (j * 4 + tap) * 8 + m
                                return vecs[:, cidx:cidx + 1]
                            def qk_proj(half):
                                if pi == 0:
                                    S.op("dve", lambda e: e.memset(qkpre[:, :, 0:3], 0.0), writes=[("qkpre", a) for a in range(4)])
                                else:
                                    S.op("dve", lambda e, half=half: e.tensor_copy(out=qkpre[:, :, 0:3], in_=halo_o[:, half * 4:half * 4 + 4, 0:3]),
                                         reads=[("halo_o", half)], writes=[("qkpre", a) for a in range(4)])

                                def cons_qk(mi, b):
                                    S.op("act", lambda e, mi=mi, b=b: e.activation(out=qkpre[:, mi, 3:515], in_=ps[b][:], func=AF.Copy),
                                         reads=[("ps", b)], writes=[("qkpre", mi)])
                                proj_fm(wsrc, 3080, half * 512, 512, lambda k: hT[:, k, :], hTr, 8, cons_qk, kfirst=(half == 0))
                                S.op("dve", lambda e, half=half: e.tensor_copy(out=halo_o[:, half * 4:half * 4 + 4, 0:3], in_=qkpre[:, :, 512:515]),
                                     reads=[("qkpre", a) for a in range(4)], writes=[("halo_o", half)])

                            def qk_conv(half):
                                for mi in range(4):
                                    m = half * 4 + mi
                                    acc, accr = ((tA, "tA"), (rstd, "rstd"))[mi % 2]
                                    S.op("dve", lambda e, mi=mi, m=m, acc=acc: e.tensor_scalar(out=acc[:], in0=qkpre[:, mi, 3:515], scalar1=cw(0, m), scalar2=None, op0=ALU.mult),
                                         reads=[("qkpre", mi), "vecs"], writes=[accr])
                                    for tap in range(1, 4):
                                        S.op("dve", lambda e, mi=mi, m=m, tap=tap, acc=acc: e.scalar_tensor_tensor(out=acc[:], in0=qkpre[:, mi, 3 - tap:515 - tap], scalar=cw(tap, m),
                                                                                                         in1=acc[:], op0=ALU.mult, op1=ALU.add),
                                             reads=[("qkpre", mi), accr, "vecs"], writes=[accr])
                                    if half == 1:
                                        S.op("act", lambda e, m=m, acc=acc: e.activation(out=qkT[:, m, :], in_=acc[:], func=AF.Silu), reads=[accr], writes=[("qkT", m)])
                                    else:
                                        t = tB[mi % 2]
                                        S.op("act", lambda e, t=t, acc=acc: e.activation(out=t[:], in_=acc[:], func=AF.Silu), reads=[accr], writes=[("tB", mi % 2)])
                                        S.op("dve", lambda e, t=t, m=m: e.tensor_scalar(out=qkT[:, m, :], in0=t[:], scalar1=float(128 ** -0.5), scalar2=None, op0=ALU.mult),
                                             reads=[("tB", mi % 2)], writes=[("qkT", m)])

                            def k_transposes():
                                for h in range(4):
                                    b = nb()
                                    for c in range(4):
                                        S.op("pe", lambda e, b=b, c=c, h=h: e.transpose(out=psb[b][:, c * 128:(c + 1) * 128], in_=qkT[:, 4 + h, c * 128:(c + 1) * 128], identity=identb[:]),
                                             reads=[("qkT", 4 + h), "identb"], writes=[("ps", b)], inc=(c == 3))
                                    S.op("dve", lambda e, b=b, h=h: e.tensor_copy(out=ktok[:, :, h * 128:(h + 1) * 128], in_=psb[b][:, 0:512].rearrange("p (c f) -> p c f", c=4)),
                                         reads=[("ps", b)], writes=[("ktok", h)])

                            def v_proj():
                                S.op("dve", lambda e: e.memset(vaug[:, :, :, 256:258], 1.0), writes=["vones"])
                                for hh in range(4):
                                    wv, wr = wload(wsrc[:, 1024 + hh * 256:1024 + (hh + 1) * 256].rearrange("(k p) n -> p k n", p=128), v3(8, 256))
                                    for tk in range(4):
                                        b = nb()
                                        MM(b, 0, 256, [(hT[:, k, tk * 128:(tk + 1) * 128], wv[:, k, :]) for k in range(8)], reads=[wr],
                                           preads=[[("hT", k)] for k in range(8)])
                                        S.op("act", lambda e, b=b, tk=tk, hh=hh: e.activation(out=vaug[:, tk, hh, 0:256], in_=ps[b][:, 0:256], func=AF.Copy),
                                             reads=[("ps", b)], writes=[("vaug", tk, hh)])

                            def o_proj():
                                for hh in range(4):
                                    wv, wr = wload(wsrc[:, 2048 + hh * 256:2048 + (hh + 1) * 256].rearrange("(k p) n -> p k n", p=128), v3(8, 256))
                                    for tk in range(4):
                                        b = nb()
                                        MM(b, 0, 256, [(hT[:, k, tk * 128:(tk + 1) * 128], wv[:, k, :]) for k in range(8)], reads=[wr],
                                           preads=[[("hT", k)] for k in range(8)])
                                        t = tB[tk % 2]
                                        S.op("act", lambda e, b=b, t=t: e.activation(out=t[:, 0:256], in_=ps[b][:, 0:256], func=AF.Sigmoid),
                                             reads=[("ps", b)], writes=[("tB", tk % 2)])
                                        S.op("dve", lambda e, t=t, tk=tk, hh=hh: e.tensor_tensor(out=sigo[:, tk, hh * 256:(hh + 1) * 256], in0=t[:, 0:256],
                                                                                             in1=lbc[:, hh * 256:(hh + 1) * 256], op=ALU.mult),
                                             reads=[("tB", tk % 2), "lbc"], writes=[("sigo", tk, hh)])

                            G = lambda i: ybuf[0:4, i, :]
                            Gr = lambda i: ("ybuf", i)
                            gbank = []

                            def gates_mm():
                                wg, wgr = wload(wsrc[:, 3072:3080].rearrange("(k p) n -> p k n", p=128), v3(8, 8))
                                bA = nb()
                                MM(bA, 0, 512, [(wg[:, k, 0:4], hT[:, k, :]) for k in range(8)], reads=[wgr] + hTr, rows=4)
                                bB = nb()
                                MM(bB, 0, 512, [(wg[:, k, 4:8], hT[:, k, :]) for k in range(8)], reads=[wgr] + hTr, rows=4)
                                gbank.extend([bA, bB])

                            def gates_part1():
                                bA, bB = gbank
                                S.op("dve", lambda e: e.tensor_scalar(out=G(0), in0=ps[bA][0:4, :], scalar1=vecs[0:4, 232 + j:233 + j], scalar2=None, op0=ALU.add),
                                     reads=[("ps", bA), "vecs"], writes=[Gr(0)])
                                S.op("act", lambda e: e.activation(out=G(1), in_=ps[bB][0:4, :], func=AF.Exp, scale=-1.0, bias=nbf[0:4, j:j + 1]),
                                     reads=[("ps", bB), "nbf"], writes=[Gr(1)])
                                S.op("act", lambda e: e.activation(out=G(1), in_=G(1), func=AF.Ln, bias=1.0), reads=[Gr(1)], writes=[Gr(1)])
                                for c in range(4):
                                    cs = slice(c * 128, (c + 1) * 128)
                                    S.op("dve", lambda e, cs=cs: e.tensor_tensor_scan(out=G(2)[:, cs], data0=ones4, data1=G(1)[:, cs], initial=0.0, op0=ALU.mult, op1=ALU.add),
                                         reads=[Gr(1), "cst"], writes=[Gr(2)])
                                S.op("dve", lambda e: e.tensor_tensor(out=G(3), in0=G(0), in1=G(2), op=ALU.add), reads=[Gr(0), Gr(2)], writes=[Gr(3)])
                                if pi == 0:
                                    S.op("dve", lambda e: e.memset(mprev[0:4, 0:1], 0.0), writes=["mprev"])
                                else:
                                    S.op("dve", lambda e: e.tensor_copy(out=mprev[0:4, 0:1], in_=mprev[0:4, 4:5]), reads=["mprev"], writes=["mprev"])

                            def gates_part2():
                                for c in range(4):
                                    cs = slice(c * 128, (c + 1) * 128)
                                    S.op("dve", lambda e, cs=cs, c=c: e.tensor_tensor_scan(out=G(4)[:, cs], data0=ones4, data1=G(3)[:, cs], initial=mprev[0:4, c:c + 1],
                                                                                        op0=ALU.mult, op1=ALU.max),
                                         reads=[Gr(3), "cst", "mprev"], writes=[Gr(4)])
                                    S.op("dve", lambda e, c=c: e.tensor_tensor(out=mprev[0:4, c + 1:c + 2], in0=G(4)[:, c * 128 + 127:c * 128 + 128],
                                                                              in1=G(2)[:, c * 128 + 127:c * 128 + 128], op=ALU.subtract),
                                         reads=[Gr(4), Gr(2), "mprev"], writes=["mprev"])
                                    S.op("dve", lambda e, c=c: e.tensor_scalar(out=smal[0:4, 8 + c:9 + c], in0=G(4)[:, c * 128 + 127:c * 128 + 128], scalar1=-1.0, scalar2=None, op0=ALU.mult),
                                         reads=[Gr(4)], writes=[("smal", 8)])

                            def gates_part3():
                                for c in range(4):
                                    cs = slice(c * 128, (c + 1) * 128)
                                    S.op("act", lambda e, cs=cs, c=c: e.activation(out=G(5)[:, cs], in_=G(4)[:, cs], func=AF.Exp, scale=-1.0, bias=mprev[0:4, c:c + 1]),
                                         reads=[Gr(4), "mprev"], writes=[Gr(5)])
                                    S.op("act", lambda e, cs=cs, c=c: e.activation(out=G(7)[:, cs], in_=G(3)[:, cs], func=AF.Exp, bias=smal[0:4, 8 + c:9 + c]),
                                         reads=[Gr(3), ("smal", 8)], writes=[Gr(7)])
                                S.op("dve", lambda e: e.tensor_tensor(out=G(6), in0=G(2), in1=G(4), op=ALU.subtract), reads=[Gr(2), Gr(4)], writes=[Gr(6)])
                                S.op("act", lambda e: e.activation(out=G(6), in_=G(6), func=AF.Exp), reads=[Gr(6)], writes=[Gr(6)])
                                S.op("dve", lambda e: e.tensor_tensor(out=smal[0:4, 12:16], in0=mprev[0:4, 0:4], in1=smal[0:4, 8:12], op=ALU.add),
                                     reads=["mprev", ("smal", 8)], writes=[("smal", 12)])
                                S.op("act", lambda e: e.activation(out=smal[0:4, 12:16], in_=smal[0:4, 12:16], func=AF.Exp), reads=[("smal", 12)], writes=[("smal", 12)])
                                S.op("dve", lambda e: e.tensor_scalar(out=G(4), in0=G(4), scalar1=-1.0, scalar2=None, op0=ALU.mult), reads=[Gr(4)], writes=[Gr(4)])
                                bt = nb()
                                for qi, gi in enumerate((3, 5, 6, 7)):
                                    for c in range(4):
                                        col = (qi * 4 + c) * 4
                                        S.op("pe", lambda e, gi=gi, c=c, col=col: e.transpose(out=ps[bt][:, col:col + 4], in_=ybuf[0:4, gi, c * 128:(c + 1) * 128], identity=cst[0:4, 0:4]),
                                             reads=[Gr(gi), "cst"], writes=[("ps", bt)], inc=(qi == 3 and c == 3))
                                S.op("dve", lambda e: e.tensor_copy(out=tokq, in_=ps[bt][:, 0:64]), reads=[("ps", bt)], writes=["tokq"])
                                bw = nb()
                                for h in range(4):
                                    S.op("pe", lambda e, h=h: e.matmul(ps[bw][:, h * 4:(h + 1) * 4], lhsT=cst[0:4, 768 + h * 128:768 + (h + 1) * 128], rhs=smal[0:4, 12:16], start=True, stop=True),
                                         reads=[("smal", 12), "cst"], writes=[("ps", bw)], inc=(h == 3))
                                S.op("dve", lambda e: e.tensor_copy(out=wcb, in_=ps[bw][:, 0:16]), reads=[("ps", bw)], writes=["wcb"])
                                if pi == 0:
                                    S.op("dve", lambda e: e.memset(Cst[:], 0.0), writes=[("Cst", h) for h in range(4)])
                                    S.op("dve", lambda e: e.memset(Cb[:], 0.0), writes=[("Cb", h) for h in range(4)])

                            gates_mm()
                            gates_part1()
                            qk_proj(0)
                            v_proj()
                            qk_conv(0)
                            gates_part2()
                            qk_proj(1)
                            o_proj()
                            qk_conv(1)
                            gates_part3()
                            k_transposes()
                            QK4 = [("qkpre", a) for a in range(4)]
                            for h in range(4):
                                bS = nb()
                                for c in range(4):
                                    cs = slice(c * 128, (c + 1) * 128)
                                    S.op("pe", lambda e, cs=cs, h=h, bS=bS: e.matmul(ps[bS][:, cs], lhsT=qkT[:, 4 + h, cs], rhs=qkT[:, h, cs], start=True, stop=True),
                                         reads=[("qkT", 4 + h), ("qkT", h)], writes=[("ps", bS)], inc=(c == 3))
                                bM = nb()
                                S.op("pe", lambda e, h=h, bM=bM: e.matmul(ps[bM][:, :], lhsT=cst[0:4, 768 + h * 128:768 + (h + 1) * 128], rhs=G(4), start=True, stop=False),
                                     reads=[Gr(4), "cst"], writes=[("ps", bM)], inc=False)
                                S.op("pe", lambda e, bM=bM: e.matmul(ps[bM][:, :], lhsT=ident, rhs=maskneg4, start=False, stop=True),
                                     reads=["cst"], writes=[("ps", bM)])
                                for c in range(4):
                                    cs = slice(c * 128, (c + 1) * 128)
                                    S.op("act", lambda e, cs=cs, c=c, h=h, bM=bM: e.activation(out=DT4[:, h, cs], in_=ps[bM][:, cs], func=AF.Exp, bias=tokq[:, c * 4 + h:c * 4 + h + 1]),
                                         reads=[("ps", bM), "tokq"], writes=[("DT", h)] + (QK4 if c == 0 else []))
                                S.op("dve", lambda e, bS=bS, h=h: e.tensor_tensor(out=scT4[:, h, :], in0=ps[bS][:, :], in1=DT4[:, h, :], op=ALU.mult),
                                     reads=[("ps", bS), ("DT", h)], writes=[("scT", h)])
                            PB = [0, 1, 2, 3]
                            UB = [4, 5, 6, 7]
                            DB = 4
                            den_ps = ps[DB][:, 300:308]
                            den8 = smal[:, 24:32].rearrange("p (h t) -> p h t", t=2)
                            d1 = smal[:, 32:36]
                            d2 = smal[:, 36:40]
                            rden4 = smal[:, 40:44]
                            ssn4 = smal[:, 44:48]
                            vv = smal[:, 48:52]
                            r4 = smal[:, 52:56]
                            for c in range(4):
                                cs = slice(c * 128, (c + 1) * 128)
                                wi4 = tokq[:, 16 + c * 4:16 + c * 4 + 4]
                                fl4 = tokq[:, 32 + c * 4:32 + c * 4 + 4]
                                for h in range(4):
                                    S.op("act", lambda e, c=c, h=h: e.activation(out=kw4[:, h, :], in_=ktok[:, c, h * 128:(h + 1) * 128], func=AF.Copy,
                                                                                 scale=tokq[:, 48 + c * 4 + h:48 + c * 4 + h + 1]),
                                         reads=[("ktok", h), "tokq"], writes=[("kw", h)])
                                for h in range(4):
                                    b = PB[h]
                                    S.op("pe", lambda e, b=b, h=h, cs=cs: e.matmul(ps[b][:, 0:256], lhsT=qkT[:, h, cs], rhs=Cb[:, h, 0:256], start=True, stop=True),
                                         reads=[("qkT", h), ("Cb", h)], writes=[("ps", b)], inc=False)
                                    S.op("pe", lambda e, b=b, h=h, cs=cs, c=c: e.matmul(ps[b][:, 256:512], lhsT=scT4[:, h, cs], rhs=vaug[:, c, h, 0:256], start=True, stop=True),
                                         reads=[("scT", h), ("vaug", c, h)], writes=[("ps", b)])
                                for h in range(4):
                                    S.op("pe", lambda e, h=h, cs=cs: e.matmul(ps[DB][:, 300 + 2 * h:301 + 2 * h], lhsT=qkT[:, h, cs], rhs=Cb[:, h, 256:257], start=True, stop=True),
                                         reads=[("qkT", h), ("Cb", h)], writes=[("ps", DB)], inc=False)
                                    S.op("pe", lambda e, h=h, cs=cs, c=c: e.matmul(ps[DB][:, 301 + 2 * h:302 + 2 * h], lhsT=scT4[:, h, cs], rhs=vaug[:, c, h, 256:257], start=True, stop=True),
                                         reads=[("scT", h), "vones"], writes=[("ps", DB)], inc=(h == 3))
                                S.op("act", lambda e: e.activation(out=smal[:, 24:32], in_=den_ps, func=AF.Copy), reads=[("ps", DB)], writes=[("smal", 24)])
                                for h in range(4):
                                    MM(UB[h], 0, 257, [(kw4[:, h, :], vaug[:, c, h, 0:257])], reads=[("kw", h), ("vaug", c, h), "vones"])
                                for h in range(4):
                                    b = PB[h]
                                    S.op("act", lambda e, b=b, h=h: e.activation(out=nd4[:, h, :], in_=ps[b][:, 256:512], func=AF.Copy), reads=[("ps", b)], writes=[("nd", h)])
                                    S.op("dve", lambda e, b=b, h=h, c=c: e.scalar_tensor_tensor(out=nd4[:, h, :], in0=ps[b][:, 0:256], scalar=tokq[:, 16 + c * 4 + h:16 + c * 4 + h + 1],
                                                                                             in1=nd4[:, h, :], op0=ALU.mult, op1=ALU.add),
                                         reads=[("ps", b), ("nd", h), "tokq"], writes=[("nd", h)])
                                for h in range(4):
                                    bU = UB[h]
                                    S.op("dve", lambda e, h=h, c=c, bU=bU: e.scalar_tensor_tensor(out=Cst[:, h, :], in0=Cst[:, h, :], scalar=wcb[:, h * 4 + c:h * 4 + c + 1],
                                                                                               in1=ps[bU][:, 0:257], op0=ALU.mult, op1=ALU.add),
                                         reads=[("Cst", h), "wcb", ("ps", bU)], writes=[("Cst", h)])
                                for h in range(4):
                                    S.op("act", lambda e, h=h: e.activation(out=tA[:, 0:256], in_=nd4[:, h, :], func=AF.Square, accum_out=ssn4[:, h:h + 1]),
                                         reads=[("nd", h)], writes=[("ssn", h)])
                                for h in range(4):
                                    S.op("act", lambda e, h=h: e.activation(out=Cb[:, h, 0:257], in_=Cst[:, h, :], func=AF.Copy), reads=[("Cst", h)], writes=[("Cb", h)])
                                S.op("dve", lambda e: e.tensor_tensor(out=d1, in0=den8[:, :, 0], in1=wi4, op=ALU.mult), reads=[("smal", 24), "tokq"], writes=[("smal", 32)])
                                S.op("dve", lambda e: e.tensor_tensor(out=d1, in0=d1, in1=den8[:, :, 1], op=ALU.add), reads=[("smal", 24), ("smal", 32)], writes=[("smal", 32)])
                                S.op("dve", lambda e: e.tensor_scalar(out=d2, in0=d1, scalar1=-1.0, scalar2=None, op0=ALU.mult), reads=[("smal", 32)], writes=[("smal", 36)])
                                S.op("dve", lambda e: e.tensor_tensor(out=d1, in0=d1, in1=d2, op=ALU.max), reads=[("smal", 32), ("smal", 36)], writes=[("smal", 32)])
                                S.op("dve", lambda e: e.tensor_tensor(out=d1, in0=d1, in1=fl4, op=ALU.max), reads=[("smal", 32), "tokq"], writes=[("smal", 32)])
                                S.op("dve", lambda e: e.reciprocal(out=rden4, in_=d1), reads=[("smal", 32)], writes=[("smal", 40)])
                                S.op("dve", lambda e: e.tensor_tensor(out=vv, in0=rden4, in1=rden4, op=ALU.mult), reads=[("smal", 40)], writes=[("smal", 48)])
                                S.op("dve", lambda e: e.tensor_tensor(out=vv, in0=vv, in1=ssn4, op=ALU.mult), reads=[("smal", 48)] + [("ssn", h) for h in range(4)], writes=[("smal", 48)])
                                S.op("dve", lambda e: e.tensor_scalar(out=vv, in0=vv, scalar1=1.0 / 256, scalar2=EPS, op0=ALU.mult, op1=ALU.add), reads=[("smal", 48)], writes=[("smal", 48)])
                                S.op("dve", lambda e: e.reciprocal(out=vv, in_=vv), reads=[("smal", 48)], writes=[("smal", 48)])
                                S.op("act", lambda e: e.activation(out=vv, in_=vv, func=AF.Sqrt), reads=[("smal", 48)], writes=[("smal", 48)])
                                S.op("dve", lambda e: e.tensor_tensor(out=r4, in0=vv, in1=rden4, op=ALU.mult), reads=[("smal", 48), ("smal", 40)], writes=[("smal", 52)])
                                for h in range(4):
                                    hs = slice(h * 256, (h + 1) * 256)
                                    S.op("dve", lambda e, c=c, h=h, hs=hs: e.scalar_tensor_tensor(out=sigo[:, c, hs], in0=nd4[:, h, :], scalar=smal[:, 52 + h:53 + h], in1=sigo[:, c, hs],
                                                                                               op0=ALU.mult, op1=ALU.mult),
                                         reads=[("nd", h), ("smal", 52), ("sigo", c, h)], writes=[("sigo", c, h)])
                            for c in range(4):
                                for half in range(2):
                                    b = nb()
                                    for q in range(4):
                                        ee = half * 4 + q
                                        S.op("pe", lambda e, b=b, q=q, ee=ee, c=c: e.transpose(out=psb[b][:, q * 128:(q + 1) * 128], in_=sigo[:, c, ee * 128:(ee + 1) * 128], identity=identb[:]),
                                             reads=[("sigo", c, ee // 2), "identb"], writes=[("ps", b)], inc=(q == 3))
                                    S.op("dve", lambda e, b=b, c=c, half=half: e.tensor_copy(out=hT[:, half * 4:half * 4 + 4, c * 128:(c + 1) * 128],
                                                                                          in_=psb[b][:, 0:512].rearrange("p (q t) -> p q t", q=4)),
                                         reads=[("ps", b)], writes=[("hT", half * 4 + q) for q in range(4)])
                            def cons_yo(mi, b):
                                S.op("act", lambda e, mi=mi, b=b: e.activation(out=ybuf[:, mi, :], in_=ps[b][:], func=AF.Copy),
                                     reads=[("ps", b)], writes=[("ybuf", mi)])
                                ysq(mi, b)
                            proj_fm(owout[j], 1024, 0, 1024, lambda k: hT[:, k, :], hTr, 8, cons_yo)
                        postnorm_add(1, l, pi, follow="sq")
                        S.barrier()
                        prenorm(2, l, pi, presq=True)
                        ar = Ar()
                        hid = ar.b16(22 * TP).rearrange("p (a t) -> p a t", a=22)
                        sg = [ar.f32(TP), ar.f32(TP)]
                        for j0 in range(0, 22, 2):
                            nj = 2
                            gv, gr = wload(wgu[l][:, j0 * 128:(j0 + nj) * 128].rearrange("(k p) n -> p k n", p=128), v3(8, nj * 128))
                            uv, ur = wload(wgu[l][:, 2816 + j0 * 128:2816 + (j0 + nj) * 128].rearrange("(k p) n -> p k n", p=128), v3(8, nj * 128))
                            hTk = [("hT", k) for k in range(8)]
                            bgs = [nb() for _ in range(4)]
                            if KFIRST and j0 == 0:
                                MMK(bgs, lambda i, k: (gv if i % 2 == 0 else uv)[:, k, (i // 2) * 128:(i // 2 + 1) * 128], lambda k: hT[:, k, :], 8,
                                    [gr, ur, gr, ur], hTk)
                            else:
                                for i, b in enumerate(bgs):
                                    wv_, wr_ = (gv, gr) if i % 2 == 0 else (uv, ur)
                                    MM(b, 0, TP, [(wv_[:, k, (i // 2) * 128:(i // 2 + 1) * 128], hT[:, k, :]) for k in range(8)],
                                       reads=[wr_], preads=[[hTk[k]] for k in range(8)])
                            for jj in range(nj):
                                jx = j0 + jj
                                bg, bu = bgs[2 * jj], bgs[2 * jj + 1]
                                sgt = sg[jx % 2]
                                S.op("act", lambda e, bg=bg, sgt=sgt: e.activation(out=sgt, in_=ps[bg][:], func=AF.Silu),
                                     reads=[("ps", bg)], writes=[("sg", jx % 2)])
                                S.op("dve", lambda e, bu=bu, sgt=sgt, jx=jx: e.tensor_tensor(out=hid[:, jx, :], in0=ps[bu][:], in1=sgt, op=ALU.mult),
                                     reads=[("ps", bu), ("sg", jx % 2)], writes=[("hid", jx)])
                        for m in range(8):
                            dvs = []
                            for jh in range(2):
                                dv, dr = wload(wdn[l][jh * 1408:(jh + 1) * 1408, m * 128:(m + 1) * 128].rearrange("(j p) n -> p j n", p=128), v3(11, 128))
                                dvs.append((dv, dr))
                            b = nb()
                            MM(b, 0, TP, [(dvs[jx // 11][0][:, jx % 11, :], hid[:, jx, :]) for jx in range(22)], reads=[],
                               preads=[[dvs[jx // 11][1], ("hid", jx)] for jx in range(22)])
                            S.op("act", lambda e, m=m, b=b: e.activation(out=ybuf[:, m, :], in_=ps[b][:], func=AF.Copy),
                                 reads=[("ps", b)], writes=[("ybuf", m)])
                            ysq(m, b)
                        postnorm_add(3, l, pi, follow="cast")
                        ar = Ar()
                        ar.off = 6656
                        sgp = [ar.f32(TP), ar.f32(TP)]
                        for kk in range(2):
                            b = nb()
                            for c in range(4):
                                S.op("pe", lambda e, b=b, c=c, kk=kk: e.transpose(out=psb[b][:, c * 128:(c + 1) * 128], in_=pin[:, c, kk * 128:(kk + 1) * 128],
                                                                                 identity=identb[:]),
                                     reads=["pin", "identb"], writes=[("ps", b)], inc=(c == 3))
                            S.op("dve", lambda e, b=b, kk=kk: e.tensor_copy(out=pT[:, kk, :], in_=psb[b][:, 0:512]), reads=[("ps", b)], writes=[("pT", kk)])
                        prv, prr = wload(pproj[l].rearrange("(k p) n -> p k n", p=128), v3(2, 1024))
                        for mi in range(8):
                            b2 = nb()
                            MM(b2, 0, TP, [(prv[:, k, mi * 128:(mi + 1) * 128], pT[:, k, :]) for k in range(2)],
                               reads=[prr, ("pT", 0), ("pT", 1)])
                            S.op("act", lambda e, b2=b2, mi=mi: e.activation(out=ybuf[:, mi, :], in_=ps[b2][:], func=AF.Copy),
                                 reads=[("ps", b2)], writes=[("ybuf", mi)])

                        def cons_e(mi, b):
                            sgt = sgp[mi % 2]
                            S.op("act", lambda e, b=b, sgt=sgt: e.activation(out=sgt, in_=ps[b][:], func=AF.Sigmoid),
                                 reads=[("ps", b)], writes=[("sgp", mi % 2)])
                            S.op("dve", lambda e, sgt=sgt, mi=mi: e.tensor_tensor(out=ybuf[:, mi, :], in0=ybuf[:, mi, :], in1=sgt, op=ALU.mult),
                                 reads=[("ybuf", mi), ("sgp", mi % 2)], writes=[("ybuf", mi)])
                            S.op("dve", lambda e, mi=mi: e.tensor_tensor(out=sq[:, mi, :], in0=ybuf[:, mi, :], in1=ybuf[:, mi, :], op=ALU.mult),
                                 reads=[("ybuf", mi)], writes=[("sq", mi)])
                        proj_fm(pgate[l], 1024, 0, 1024, lambda k: hT[:, k, :], [("hT", k) for k in range(8)], 8, cons_e, kfirst=True)
                        postnorm_add(4, l, pi)

                    for pi in range(NPASS):
                        do_pass(s, l, j, even, pi)

                S.barrier()
                ar = Ar()
                xo = [ar.f32(1024), ar.f32(1024)]
                def store_tile(s, tt):
                    xi = xo[tt % 2]
                    pi_ = (tt * 128) // TP
                    for half in range(2):
                        b = nb()
                        for q in range(4):
                            c = half * 4 + q
                            S.op("pe", lambda e, c=c, q=q, b=b: e.transpose(out=ps[b][:, q * 128:(q + 1) * 128],
                                                                          in_=xT[:, c, tt * 128:(tt + 1) * 128], identity=ident),
                                 reads=[("x", c, pi_), "cst"], writes=[("ps", b)], inc=(q == 3))
                        if half == 0:
                            S.op("dve", lambda e, b=b, xi=xi: e.tensor_copy(out=xi[:, 0:512], in_=ps[b][:]), reads=[("ps", b)],
                                 writes=[("xo", tt % 2, 0)])
                        else:
                            S.op("act", lambda e, b=b, xi=xi: e.activation(out=xi[:, 512:1024], in_=ps[b][:], func=AF.Copy), reads=[("ps", b)],
                                 writes=[("xo", tt % 2, 1)])
                    out_evs.append(S.dma("sp", out_d[s, tt * 128:(tt + 1) * 128, :], xi, reads=[("xo", tt % 2, 0), ("xo", tt % 2, 1)]))

                for tt in range(NTT):
                    store_tile(s, tt)

            S.finish("sp", out_evs)
            return wlist

        S0 = Sched(nc, sems)
        plan = record(S0, None)
        S = Sched(nc, sems)
        record(S, plan)
        S.emit()
    return nc


def _consts():
    c = np.zeros((128, NCONST), np.float32)
    c[:, 0:128] = np.eye(128, dtype=np.float32)
    s = np.arange(128)[:, None]
    t = np.arange(128)[None, :]
    tri = (s <= t).astype(np.float32)
    c[:, 128:256] = tri
    c[:, 256:768] = np.tile(np.where(s <= t, 0.0, NEGBIG).astype(np.float32), (1, 4))
    for h in range(4):
        c[h, 768 + h * 128:768 + (h + 1) * 128] = 1.0
    for g, win in enumerate((2, 4, 8, 16)):
        c[:, 1280 + g * 16:1280 + (g + 1) * 16] = (1.0 / np.minimum(np.arange(16) + 1, win)).astype(np.float32)[None, :]
    c[:, 1344:1472] = 1.0
    return c


def _prep_shared(inp):
    f = lambda a: np.ascontiguousarray(np.asarray(a, dtype=np.float32))
    vecs = np.zeros((128, NV), np.float32)
    kinds = ["mix_pre_gain", "mix_post_gain", "ffn_pre_gain", "ffn_post_gain", "ple_post_gain"]
    for k, name in enumerate(kinds):
        g = f(inp[name])
        vecs[:, k * 32:(k + 1) * 32] = g.reshape(4, 8, 128).transpose(2, 0, 1).reshape(128, 32)
    vecs[:, 160:168] = f(inp["even_b_scale"]).reshape(2, 4, 128).transpose(2, 0, 1).reshape(128, 8)
    vecs[:, 168:232] = f(inp["odd_conv_w"]).reshape(2, 4, 8, 128).transpose(3, 0, 1, 2).reshape(128, 64)
    vecs[0:4, 232:234] = f(inp["odd_b_i"]).T
    vecs[0:4, 234:236] = f(inp["odd_b_f"]).T
    bce = np.zeros((2, 128, 2560), np.float32)
    bce[:, :, 0:512] = f(inp["even_a_v_gain"])[:, None, :]
    bs = f(inp["even_a_bs"])
    bce[:, :, 512:2560] = np.tile(bs[:, :, None, :], (1, 1, 4, 1)).reshape(2, 1, 2048)
    bco = np.ascontiguousarray(np.broadcast_to(f(inp["odd_h_gain"])[:, None, :], (2, 128, 1024)))
    d = {
        "even_w_in": f(inp["even_w_in"]), "even_w_out": f(inp["even_w_out"]), "even_b_wpool": f(inp["even_b_wpool"]),
        "ws_t": np.ascontiguousarray(f(inp["even_a_ws"]).transpose(0, 1, 3, 2)),
        "odd_w_in": f(inp["odd_w_in"]), "odd_w_out": f(inp["odd_w_out"]),
        "ffn_w_gate_up": f(inp["ffn_w_gate_up"]), "ffn_w_down": f(inp["ffn_w_down"]),
        "ple_proj": f(inp["ple_proj"]), "ple_gate": f(inp["ple_gate"]),
        "vecs": vecs, "bc_even": bce, "bc_odd": bco, "consts": _consts(),
    }
    return d


def run(inp, n_cores, NSEQ, T, NL, trace=False):
    x = np.asarray(inp["x"], dtype=np.float32)
    p = np.asarray(inp["p"], dtype=np.float32)
    shared = _prep_shared(inp)
    nc = build_nc(NSEQ, T, NL)
    in_maps = []
    for c in range(n_cores):
        m = dict(shared)
        m["x"] = np.ascontiguousarray(x[c * NSEQ:(c + 1) * NSEQ])
        m["p"] = np.ascontiguousarray(p[:, c * NSEQ:(c + 1) * NSEQ])
        in_maps.append(m)
    res = run_bass_kernel_spmd(nc, in_maps, core_ids=list(range(n_cores)), trace=trace)
    out = np.concatenate([r["out"] for r in res.results], axis=0)
    return out, res


def kernel(**inputs):
    out, _ = run(inputs, 8, 2, 2048, 4)
    return out.astype(np.float32)
```
